# Optimizing a Trainium2 kernel written in Bass

```python
import math
import jax
import jax.numpy as jnp
from jax import lax
import numpy as np

D_MODEL = 1024
BATCH = 16
SEQ = 256
DEPTH = 1
DEC_BATCH = 8
DEC_SEQ = 1024
PAST_LEN = 256

GRID_W = 64
POS_BASE = 10000.0
D_MIX = D_MODEL
D_HY = D_MIX // 2
D_S5 = D_MIX - D_HY
S5_CH = 16
S5_GROUPS = D_S5 // S5_CH
S5_STATE = 64
HY_ORDER = 2
HY_SHORT = 3
HY_BANDS = 16
HY_EMB = 1 + 2 * HY_BANDS
HY_HID = 64
HY_MIN_DECAY = math.log(1e-2) / 1.5
HY_MAX_DECAY = math.log(1e-2) / 0.3
N_EGROUPS = 4
N_EPG = 4
N_EXPERTS = N_EGROUPS * N_EPG
TOP_K_INNER = 2
D_EXPERT = 512
LN_EPS = 1e-5
F32 = jnp.float32

kernel_name = 'hyena_s5_hmoe_diffusion_step'


def _norm(x):
    xf = x.astype(F32)
    xc = xf - jnp.mean(xf, axis=-1, keepdims=True)
    return xc * lax.rsqrt(jnp.mean(xc * xc, axis=-1, keepdims=True) + LN_EPS)


def _ln_affine(x, g, b):
    return _norm(x) * g.astype(F32) + b.astype(F32)


def _rms(y):
    yf = y.astype(F32)
    return yf * lax.rsqrt(jnp.mean(yf * yf, axis=-1, keepdims=True) + LN_EPS)


def _grid_pos_embed(n_tokens):
    rows = n_tokens // GRID_W
    row = jnp.repeat(jnp.arange(rows, dtype=F32), GRID_W)
    col = jnp.tile(jnp.arange(GRID_W, dtype=F32), rows)
    quarter = D_MODEL // 4
    omega = 1.0 / (POS_BASE ** (jnp.arange(quarter, dtype=F32) / quarter))
    er = row[:, None] * omega
    ec = col[:, None] * omega
    return jnp.concatenate([jnp.sin(er), jnp.cos(er), jnp.sin(ec), jnp.cos(ec)], axis=-1)


def _hyena_pos_features(n_tok):
    t = jnp.linspace(0.0, 1.0, n_tok, dtype=F32)[:, None]
    w = 2.0 * math.pi * jnp.arange(n_tok, dtype=F32) / n_tok
    f = jnp.linspace(1e-4, HY_BANDS - 1, HY_BANDS, dtype=F32)
    ang = w[:, None] * f[None, :]
    z = jnp.concatenate([t, jnp.cos(ang), -jnp.sin(ang)], axis=-1)
    return z, t


def _hyena_filter_spectrum(n_tok, w1, b1, w2, b2, w3, freq):
    z, t = _hyena_pos_features(n_tok)
    fr = freq.astype(F32)
    h = jnp.sin(fr * (z @ w1.astype(F32) + b1.astype(F32)))
    h = jnp.sin(fr * (h @ w2.astype(F32) + b2.astype(F32)))
    h = (h @ w3.astype(F32)).reshape(n_tok, HY_ORDER, 2, D_HY)
    deltas = jnp.linspace(HY_MIN_DECAY, HY_MAX_DECAY, D_HY, dtype=F32)
    h = h * jnp.exp(-t[:, :, None, None] * jnp.abs(deltas))
    h_fwd = h[:, :, 0]
    h_bwd = h[:, :, 1]
    k = jnp.concatenate([h_fwd, jnp.zeros_like(h_fwd[:1]), h_bwd[:0:-1]], axis=0)
    return jnp.fft.rfft(k, axis=0)


def _fftconv(u, k_f, d):
    n_tok = u.shape[1]
    U = jnp.fft.rfft(u, n=2 * n_tok, axis=1)
    y = jnp.fft.irfft(U * k_f[None], n=2 * n_tok, axis=1)[:, :n_tok]
    return y + u * d.astype(F32)


def _hyena(proj, conv_w, conv_b, kf, fbias):
    n_tok = proj.shape[1]
    pad = HY_SHORT // 2
    p = jnp.pad(proj.astype(F32), ((0, 0), (pad, pad), (0, 0)))
    cw = conv_w.astype(F32)
    s = conv_b.astype(F32)
    for j in range(HY_SHORT):
        s = s + p[:, j:j + n_tok] * cw[j]
    v, x1, x2 = jnp.split(s, 3, axis=-1)
    z = x1 * _fftconv(v, kf[:, 0], fbias[0])
    return x2 * _fftconv(z, kf[:, 1], fbias[1])


def _zoh(a_re, a_im, log_dt, b_re, b_im):
    A = lax.complex(a_re.astype(F32), a_im.astype(F32))
    dt = jnp.exp(log_dt.astype(F32))[:, None]
    a_bar = jnp.exp(A * dt)
    Bm = lax.complex(b_re.astype(F32), b_im.astype(F32))
    b_bar = ((a_bar - 1.0) / A)[..., None] * Bm
    return a_bar, b_bar


def _ssm_binop(e1, e2):
    a1, b1 = e1
    a2, b2 = e2
    return a1 * a2, a2 * b1 + b2


def _s5_scan(a_bar, bu, h0):
    a = jnp.broadcast_to(a_bar, bu.shape)
    a_cum, xs = lax.associative_scan(_ssm_binop, (a, bu), axis=1)
    xs = xs + a_cum * h0[:, None]
    return xs, xs[:, -1]


def _s5(u, h0_f, h0_b, a_re, a_im, log_dt, b_re, b_im, c_re, c_im, d, w_glu, b_glu):
    bsz, n_tok, _ = u.shape
    uf = u.astype(F32).reshape(bsz, n_tok, S5_GROUPS, S5_CH)
    uc = uf.astype(jnp.complex64)
    af, bf = _zoh(a_re[0], a_im[0], log_dt[0], b_re[0], b_im[0])
    ab, bb = _zoh(a_re[1], a_im[1], log_dt[1], b_re[1], b_im[1])
    xs_f, h_f = _s5_scan(af, jnp.einsum('blgh,gph->blgp', uc, bf), h0_f)
    xs_b_rev, h_b = _s5_scan(ab, jnp.einsum('blgh,gph->blgp', uc[:, ::-1], bb), h0_b)
    states = xs_f + xs_b_rev[:, ::-1]
    C = lax.complex(c_re.astype(F32), c_im.astype(F32))
    y = jnp.einsum('blgp,ghp->blgh', states, C).real + uf * d.astype(F32).reshape(S5_GROUPS, S5_CH)
    y = y.reshape(bsz, n_tok, D_S5)
    y = jax.nn.gelu(y) * jax.nn.sigmoid(y @ w_glu.astype(F32) + b_glu.astype(F32))
    return y, h_f, h_b


def _hier_moe(h, w_r1, b_r1, w_r2, b_r2, w_gate, w_up, w_down):
    bsz, n_tok, dm = h.shape
    t = h.reshape(bsz * n_tok, dm)
    tf = t.astype(F32)
    n = t.shape[0]
    rows = jnp.arange(n)
    l1 = tf @ w_r1.astype(F32) + b_r1.astype(F32)
    p1 = jax.nn.softmax(l1, axis=-1)
    grp = jnp.argmax(l1, axis=-1)
    p_grp = p1[rows, grp][:, None]
    l2_all = jnp.einsum('td,gde->tge', tf, w_r2.astype(F32)) + b_r2.astype(F32)
    l2 = l2_all[rows, grp]
    top_v, top_i = lax.top_k(l2, TOP_K_INNER)
    w_sel = jax.nn.softmax(top_v, axis=-1) * p_grp
    eid = grp[:, None] * N_EPG + top_i
    gates = jnp.sum(jax.nn.one_hot(eid, N_EXPERTS, dtype=F32) * w_sel[..., None], axis=1)
    a = jnp.einsum('td,edf->tef', t, w_gate)
    u = jnp.einsum('td,edf->tef', t, w_up)
    hid = jax.nn.silu(a) * u * gates[:, :, None].astype(t.dtype)
    y = jnp.einsum('tef,efd->td', hid, w_down)
    return y.reshape(bsz, n_tok, dm)


def _layer(x, cond, h0_f, h0_b, alpha, lp):
    n_tok = x.shape[1]
    mod = (cond @ lp['w_ada'].astype(F32) + lp['b_ada'].astype(F32))[:, None, :]
    sh1, sc1, g1, sh2, sc2, g2 = jnp.split(mod, 6, axis=-1)
    h = (_norm(x) * (1.0 + sc1) + sh1).astype(x.dtype)
    proj = h @ lp['w_in']
    kf = _hyena_filter_spectrum(n_tok, lp['hy_f_w1'], lp['hy_f_b1'], lp['hy_f_w2'],
                                lp['hy_f_b2'], lp['hy_f_w3'], lp['hy_freq'])
    y_hy = _hyena(proj[..., :3 * D_HY], lp['hy_conv_w'], lp['hy_conv_b'], kf, lp['hy_fbias'])
    y_s5, h_f, h_b = _s5(proj[..., 3 * D_HY:], h0_f, h0_b, lp['s5_a_re'], lp['s5_a_im'],
                         lp['s5_log_dt'], lp['s5_b_re'], lp['s5_b_im'], lp['s5_c_re'],
                         lp['s5_c_im'], lp['s5_d'], lp['s5_w_glu'], lp['s5_b_glu'])
    mixed = jnp.concatenate([_rms(y_hy), _rms(y_s5)], axis=-1) * lp['out_norm_g'].astype(F32)
    o = mixed.astype(x.dtype) @ lp['w_out']
    x = _ln_affine(alpha * x.astype(F32) + g1 * o.astype(F32), lp['ln1_g'], lp['ln1_b']).astype(x.dtype)
    h = (_norm(x) * (1.0 + sc2) + sh2).astype(x.dtype)
    f = _hier_moe(h, lp['moe_w_r1'], lp['moe_b_r1'], lp['moe_w_r2'], lp['moe_b_r2'],
                  lp['moe_w_gate'], lp['moe_w_up'], lp['moe_w_down'])
    x = _ln_affine(alpha * x.astype(F32) + g2 * f.astype(F32), lp['ln2_g'], lp['ln2_b']).astype(x.dtype)
    return x, h_f, h_b


def setup_inputs(seed: int = 0) -> dict:
    key = jax.random.key(seed)
    ks = iter(jax.random.split(key, 48))

    def nrm(shape, scale):
        return scale * jax.random.normal(next(ks), shape, F32)

    beta = (8.0 * DEPTH) ** -0.25
    n_idx = jnp.arange(S5_STATE, dtype=F32)
    return {
        'x_prompt': nrm((BATCH, SEQ, D_MODEL), 1.0),
        'x_sample': nrm((DEC_BATCH, DEC_SEQ, D_MODEL), 1.0),
        'state_s5_re': nrm((DEC_BATCH, DEPTH, 2, S5_GROUPS, S5_STATE), 0.3),
        'state_s5_im': nrm((DEC_BATCH, DEPTH, 2, S5_GROUPS, S5_STATE), 0.3),
        'c': nrm((DEC_BATCH, D_MODEL), 1.0),
        'c_ctx': nrm((D_MODEL,), 1.0),
        'w_ada': nrm((DEPTH, D_MODEL, 6 * D_MODEL), D_MODEL ** -0.5),
        'b_ada': nrm((DEPTH, 6 * D_MODEL), 0.02),
        'w_in': nrm((DEPTH, D_MODEL, 3 * D_HY + D_S5), D_MODEL ** -0.5),
        'hy_conv_w': nrm((DEPTH, HY_SHORT, 3 * D_HY), HY_SHORT ** -0.5),
        'hy_conv_b': nrm((DEPTH, 3 * D_HY), 0.01),
        'hy_f_w1': nrm((DEPTH, HY_EMB, HY_HID), HY_EMB ** -0.5),
        'hy_f_b1': nrm((DEPTH, HY_HID), 0.02),
        'hy_f_w2': nrm((DEPTH, HY_HID, HY_HID), HY_HID ** -0.5),
        'hy_f_b2': nrm((DEPTH, HY_HID), 0.02),
        'hy_f_w3': nrm((DEPTH, HY_HID, HY_ORDER * 2 * D_HY), 0.3 * HY_HID ** -0.5),
        'hy_freq': 1.0 + nrm((DEPTH, HY_HID), 0.02),
        'hy_fbias': nrm((DEPTH, HY_ORDER, D_HY), 1.0),
        's5_a_re': -0.5 + nrm((DEPTH, 2, S5_GROUPS, S5_STATE), 0.01),
        's5_a_im': math.pi * n_idx + nrm((DEPTH, 2, S5_GROUPS, S5_STATE), 0.01),
        's5_log_dt': jax.random.uniform(next(ks), (DEPTH, 2, S5_GROUPS), F32,
                                        minval=math.log(1e-3), maxval=math.log(1e-1)),
        's5_b_re': nrm((DEPTH, 2, S5_GROUPS, S5_STATE, S5_CH), (2.0 * S5_CH) ** -0.5),
        's5_b_im': nrm((DEPTH, 2, S5_GROUPS, S5_STATE, S5_CH), (2.0 * S5_CH) ** -0.5),
        's5_c_re': nrm((DEPTH, S5_GROUPS, S5_CH, S5_STATE), (2.0 * S5_STATE) ** -0.5),
        's5_c_im': nrm((DEPTH, S5_GROUPS, S5_CH, S5_STATE), (2.0 * S5_STATE) ** -0.5),
        's5_d': nrm((DEPTH, D_S5), 1.0),
        's5_w_glu': nrm((DEPTH, D_S5, D_S5), D_S5 ** -0.5),
        's5_b_glu': nrm((DEPTH, D_S5), 0.02),
        'out_norm_g': 1.0 + nrm((DEPTH, D_MIX), 0.02),
        'w_out': nrm((DEPTH, D_MIX, D_MODEL), beta * D_MIX ** -0.5),
        'ln1_g': 1.0 + nrm((DEPTH, D_MODEL), 0.02),
        'ln1_b': nrm((DEPTH, D_MODEL), 0.02),
        'moe_w_r1': nrm((DEPTH, D_MODEL, N_EGROUPS), D_MODEL ** -0.5),
        'moe_b_r1': nrm((DEPTH, N_EGROUPS), 0.01),
        'moe_w_r2': nrm((DEPTH, N_EGROUPS, D_MODEL, N_EPG), D_MODEL ** -0.5),
        'moe_b_r2': nrm((DEPTH, N_EGROUPS, N_EPG), 0.01),
        'moe_w_gate': nrm((DEPTH, N_EXPERTS, D_MODEL, D_EXPERT), D_MODEL ** -0.5),
        'moe_w_up': nrm((DEPTH, N_EXPERTS, D_MODEL, D_EXPERT), D_MODEL ** -0.5),
        'moe_w_down': nrm((DEPTH, N_EXPERTS, D_EXPERT, D_MODEL), beta * D_EXPERT ** -0.5),
        'ln2_g': 1.0 + nrm((DEPTH, D_MODEL), 0.02),
        'ln2_b': nrm((DEPTH, D_MODEL), 0.02),
    }


def reference(x_prompt, x_sample, state_s5_re, state_s5_im, c, c_ctx, w_ada, b_ada, w_in,
              hy_conv_w, hy_conv_b, hy_f_w1, hy_f_b1, hy_f_w2, hy_f_b2, hy_f_w3, hy_freq,
              hy_fbias, s5_a_re, s5_a_im, s5_log_dt, s5_b_re, s5_b_im, s5_c_re, s5_c_im, s5_d,
              s5_w_glu, s5_b_glu, out_norm_g, w_out, ln1_g, ln1_b, moe_w_r1, moe_b_r1,
              moe_w_r2, moe_b_r2, moe_w_gate, moe_w_up, moe_w_down, ln2_g, ln2_b):
    alpha = (2.0 * DEPTH) ** 0.25
    xp = x_prompt
    xs = (x_sample.astype(F32) + _grid_pos_embed(x_sample.shape[1])).astype(x_sample.dtype)
    ctx_cond = jax.nn.silu(c_ctx.astype(F32))[None, :]
    lat_cond = jax.nn.silu(c.astype(F32))
    new_re = []
    new_im = []
    for l in range(DEPTH):
        lp = {
            'w_ada': w_ada[l], 'b_ada': b_ada[l], 'w_in': w_in[l],
            'hy_conv_w': hy_conv_w[l], 'hy_conv_b': hy_conv_b[l],
            'hy_f_w1': hy_f_w1[l], 'hy_f_b1': hy_f_b1[l], 'hy_f_w2': hy_f_w2[l],
            'hy_f_b2': hy_f_b2[l], 'hy_f_w3': hy_f_w3[l], 'hy_freq': hy_freq[l],
            'hy_fbias': hy_fbias[l],
            's5_a_re': s5_a_re[l], 's5_a_im': s5_a_im[l], 's5_log_dt': s5_log_dt[l],
            's5_b_re': s5_b_re[l], 's5_b_im': s5_b_im[l], 's5_c_re': s5_c_re[l],
            's5_c_im': s5_c_im[l], 's5_d': s5_d[l], 's5_w_glu': s5_w_glu[l],
            's5_b_glu': s5_b_glu[l], 'out_norm_g': out_norm_g[l], 'w_out': w_out[l],
            'ln1_g': ln1_g[l], 'ln1_b': ln1_b[l],
            'moe_w_r1': moe_w_r1[l], 'moe_b_r1': moe_b_r1[l], 'moe_w_r2': moe_w_r2[l],
            'moe_b_r2': moe_b_r2[l], 'moe_w_gate': moe_w_gate[l], 'moe_w_up': moe_w_up[l],
            'moe_w_down': moe_w_down[l], 'ln2_g': ln2_g[l], 'ln2_b': ln2_b[l],
        }
        zero = jnp.zeros((xp.shape[0], S5_GROUPS, S5_STATE), jnp.complex64)
        xp, hf_ctx, hb_ctx = _layer(xp, ctx_cond, zero, zero, alpha, lp)
        st = jnp.stack([hf_ctx, hb_ctx], axis=1)
        new_re.append(st.real)
        new_im.append(st.imag)
        h0f = lax.complex(state_s5_re[:, l, 0].astype(F32), state_s5_im[:, l, 0].astype(F32))
        h0b = lax.complex(state_s5_re[:, l, 1].astype(F32), state_s5_im[:, l, 1].astype(F32))
        xs, _, _ = _layer(xs, lat_cond, h0f, h0b, alpha, lp)
    new_state_s5_re = jnp.stack(new_re, axis=1)
    new_state_s5_im = jnp.stack(new_im, axis=1)
    return (xp, xs, new_state_s5_re, new_state_s5_im)
```

```python
import math
import contextlib
import numpy as np
import ml_dtypes
import concourse.bass as bass
import concourse.mybir as mybir
from concourse.bass_utils import run_bass_kernel_spmd

F32 = mybir.dt.float32
BF16 = mybir.dt.bfloat16
F32R = mybir.dt.float32r
AF = mybir.ActivationFunctionType
ALU = mybir.AluOpType
AX = mybir.AxisListType

D = 1024
NCORE = 8
LS, LP = 1024, 256
NTOK = LS + 2 * LP
G, P, H = 32, 64, 16
T8 = 8
LN_EPS = 1e-5
ALPHA = 2.0 ** 0.25
MAGIC = 12582912.0
TWO_PI = 2.0 * math.pi


class Eng:
    def __init__(self, name, h):
        self.name, self.h = name, h
        self.sem = None
        self.cnt = 0
        self.seen = {}


class T:
    def __init__(self, ap, name):
        self.ap, self.name = ap, name
        self.w = None
        self.r = {}

    def __getitem__(self, key):
        return V(self, self.ap[key])

    @property
    def v(self):
        return V(self, self.ap)


class V:
    def __init__(self, t, ap):
        self.t, self.ap = t, ap

    def __getitem__(self, key):
        return V(self.t, self.ap[key])

    def bitcast(self, d):
        return V(self.t, self.ap.bitcast(d))

    def bc(self, shape):
        return V(self.t, self.ap.broadcast_to(shape))

    def re(self, pat, **kw):
        return V(self.t, self.ap.rearrange(pat, **kw))


def _ap(x):
    return x.ap if isinstance(x, V) else x


class KB:
    def __init__(self):
        nc = bass.Bass("TRN2", target_bir_lowering=False)
        nc.dge_precook = False
        self.nc = nc
        self.es = contextlib.ExitStack()
        self.pe = Eng("pe", nc.tensor)
        self.act = Eng("act", nc.scalar)
        self.dve = Eng("dve", nc.vector)
        self.pool = Eng("pool", nc.gpsimd)
        self.sp = Eng("sp", nc.sync)
        for e in (self.pe, self.act, self.dve, self.pool, self.sp):
            e.sem = self.es.enter_context(nc.semaphore("s_" + e.name))
        self.ndsem = 32
        self.dsem = [self.es.enter_context(nc.semaphore("s_dma%d" % i)) for i in range(self.ndsem)]
        self.dval = [0] * self.ndsem
        self.dnext = 0
        self.dnext_sw = 0
        self.out_waits = []
        self.nalloc = 0
        self.freed = []
        self.stq = self.pool
        self.npe = 0
        self.marks = []
        self.psum = [self.sb("psb%d" % i, [128, 512], F32, psum=True) for i in range(8)]
        self.psn = 0

    @contextlib.contextmanager
    def scope(self):
        st = contextlib.ExitStack()
        st._tiles = []
        try:
            yield st
        finally:
            for t in st._tiles:
                deps = {}
                for dep in ([t.w] if t.w else []) + list(t.r.values()):
                    kind, key, val = dep
                    skey = key.name if kind == "e" else "d%d" % key
                    if skey not in deps or deps[skey][2] < val:
                        deps[skey] = dep
                if deps:
                    self.freed.append((t.lo, t.hi, deps))
            st.close()

    def sb(self, name, shape, dtype, psum=False, stack=None):
        self.nalloc += 1
        nm = "%s_%d" % (name, self.nalloc)
        cm = (self.nc.psum_tensor if psum else self.nc.sbuf_tensor)(nm, shape, dtype)
        t = (stack or self.es).enter_context(cm)
        tt_ = T(t[:], nm)
        if not psum:
            ml = self.nc.lookup_mloc(nm)
            tt_.lo = int(ml.addr)
            tt_.hi = tt_.lo + int(ml.dims[1])
            keep = []
            for lo, hi, deps in self.freed:
                if lo < tt_.hi and tt_.lo < hi:
                    for skey, dep in deps.items():
                        if skey not in tt_.r or tt_.r[skey][2] < dep[2]:
                            tt_.r[skey] = dep
                    if not (tt_.lo <= lo and hi <= tt_.hi):
                        keep.append((lo, hi, deps))
                else:
                    keep.append((lo, hi, deps))
            self.freed = keep
            if stack is not None and hasattr(stack, "_tiles"):
                stack._tiles.append(tt_)
        return tt_

    def alias(self, t, name, stack):
        t2 = T(t.ap, name)
        t2.lo, t2.hi = t.lo, t.hi
        t2.r = dict(t.r)
        if stack is not None and hasattr(stack, "_tiles"):
            stack._tiles.append(t2)
        return t2

    def ps(self):
        t = self.psum[self.psn % 8]
        self.psn += 1
        return t

    def _need(self, eng, dep, waits):
        if dep is None:
            return
        kind, key, val = dep
        if kind == "e":
            if key is eng and eng is self.pe:
                return
            skey = key.name
        else:
            skey = "d%d" % key
        if eng.seen.get(skey, 0) >= val:
            return
        prev = waits.get(skey)
        if prev is None or prev[1] < val:
            waits[skey] = (key.sem if kind == "e" else self.dsem[key], val)

    def _deps(self, eng, reads, writes):
        waits = {}
        for v in reads:
            if isinstance(v, V):
                self._need(eng, v.t.w, waits)
        for v in writes:
            if isinstance(v, V):
                self._need(eng, v.t.w, waits)
                for dep in v.t.r.values():
                    self._need(eng, dep, waits)
        for skey, (sem, val) in waits.items():
            eng.h.wait_ge(sem, val)
            eng.seen[skey] = val

    def emit(self, eng, fn, reads, writes, inc=True):
        self._deps(eng, reads, writes)
        inst = fn()
        if inc:
            inst.then_inc(eng.sem, 1)
            eng.cnt += 1
            c = eng.cnt
            eng.pending = False
        else:
            c = eng.cnt + 1
            eng.pending = True
        for v in reads:
            if isinstance(v, V):
                v.t.r[eng.name] = ("e", eng, c)
        for v in writes:
            if isinstance(v, V):
                v.t.w = ("e", eng, c)
                v.t.r = {}
        return inst

    def dma(self, out, in_, q=None, is_output=False):
        is_store = isinstance(in_, V) and getattr(in_.t, "lo", None) is not None
        eng = q or (self.stq if is_store else self.sp)
        if eng is self.pool:
            i = 20 + self.dnext_sw % 12
            self.dnext_sw += 1
        else:
            i = self.dnext % 20
            self.dnext += 1
        reads = [in_] if isinstance(in_, V) else []
        writes = [out] if isinstance(out, V) else []
        self._deps(eng, reads, writes)
        if self.dval[i] > 0 and eng.seen.get("d%d" % i, 0) < self.dval[i]:
            eng.h.wait_ge(self.dsem[i], self.dval[i])
            eng.seen["d%d" % i] = self.dval[i]
        self.dval[i] += 16
        eng.h.dma_start(out=_ap(out), in_=_ap(in_)).then_inc(self.dsem[i], 16)
        dep = ("d", i, self.dval[i])
        for v in reads:
            v.t.r["d%d" % i] = dep
        for v in writes:
            v.t.w = dep
            v.t.r = {}
        if is_output:
            self.out_waits.append((i, self.dval[i]))

    def finish(self):
        for i, val in self.out_waits:
            if self.sp.seen.get("d%d" % i, 0) < val:
                self.sp.h.wait_ge(self.dsem[i], val)
                self.sp.seen["d%d" % i] = val
        for e in (self.pe, self.act, self.dve, self.pool):
            if e.cnt:
                self.sp.h.wait_ge(e.sem, e.cnt)

    def mark(self, name):
        self.marks.append((name, self.npe))

    def mm(self, out, lhsT, rhs, start=True, stop=True, inc=None):
        self.npe += 1
        if inc is None:
            inc = stop
        return self.emit(self.pe, lambda: self.nc.tensor.matmul(_ap(out), _ap(lhsT), _ap(rhs), start=start, stop=stop),
                         [lhsT, rhs], [out], inc=inc)

    def tr(self, out, in_, ident, inc=True):
        self.npe += 1
        return self.emit(self.pe, lambda: self.nc.tensor.transpose(_ap(out), _ap(in_), _ap(ident)),
                         [in_, ident], [out], inc=inc)

    def actv(self, out, in_, func, scale=1.0, bias=None, accum_out=None):
        reads = [in_] + [x for x in (scale, bias) if isinstance(x, V)]
        writes = [out] + ([accum_out] if accum_out is not None else [])
        kw = {}
        if bias is not None:
            kw["bias"] = _ap(bias)
        if accum_out is not None:
            kw["accum_out"] = _ap(accum_out)
        return self.emit(self.act, lambda: self.nc.scalar.activation(out=_ap(out), in_=_ap(in_), func=func,
                                                                      scale=_ap(scale), **kw), reads, writes)

    def tt(self, eng, out, in0, in1, op):
        return self.emit(eng, lambda: eng.h.tensor_tensor(out=_ap(out), in0=_ap(in0), in1=_ap(in1), op=op),
                         [in0, in1], [out])

    def ts(self, eng, out, in0, s1, op0, s2=None, op1=None):
        reads = [in0] + [x for x in (s1, s2) if isinstance(x, V)]
        if op1 is None:
            return self.emit(eng, lambda: eng.h.tensor_scalar(out=_ap(out), in0=_ap(in0), scalar1=_ap(s1),
                                                              scalar2=None, op0=op0), reads, [out])
        return self.emit(eng, lambda: eng.h.tensor_scalar(out=_ap(out), in0=_ap(in0), scalar1=_ap(s1),
                                                          scalar2=_ap(s2), op0=op0, op1=op1), reads, [out])

    def stt(self, eng, out, in0, scalar, in1, op0, op1):
        reads = [in0, in1] + ([scalar] if isinstance(scalar, V) else [])
        return self.emit(eng, lambda: eng.h.scalar_tensor_tensor(out=_ap(out), in0=_ap(in0), scalar=_ap(scalar),
                                                                 in1=_ap(in1), op0=op0, op1=op1), reads, [out])

    def cp(self, eng, out, in_):
        if eng is self.act:
            return self.emit(eng, lambda: self.nc.scalar.copy(out=_ap(out), in_=_ap(in_)), [in_], [out])
        return self.emit(eng, lambda: eng.h.tensor_copy(out=_ap(out), in_=_ap(in_)), [in_], [out])

    def memset(self, eng, out, val):
        return self.emit(eng, lambda: eng.h.memset(_ap(out), val), [], [out])

    def recip(self, out, in_):
        return self.emit(self.dve, lambda: self.nc.vector.reciprocal(out=_ap(out), in_=_ap(in_)), [in_], [out])

    def reduce(self, out, in_, op, axis=None):
        return self.emit(self.dve, lambda: self.nc.vector.tensor_reduce(out=_ap(out), in_=_ap(in_), axis=axis or AX.X, op=op),
                         [in_], [out])


def _bf(x):
    return np.ascontiguousarray(x.astype(ml_dtypes.bfloat16))


def _host_consts():
    c = {}
    ident = np.eye(128, dtype=np.float32)
    rr = np.arange(128) // 16
    maskF = (rr[None, :] >= rr[:, None]).astype(np.float32)
    maskB = (rr[:, None] >= rr[None, :]).astype(np.float32)
    selc = np.zeros((128, 256), np.float32)
    selc[0, 0:128] = 1.0
    selc[1, 128:256] = 1.0
    ones = np.ones((128, 128), np.float32)
    c["cst"] = np.ascontiguousarray(np.concatenate([ident, maskF, maskB, selc, ones], axis=1))
    sel = np.zeros((128, 64, 128), np.float32)
    for g8 in range(8):
        for r in range(8):
            for h in range(16):
                sel[g8 * 16 + h, g8 * 8 + r, r * 16 + h] = 1.0
    c["cstb"] = _bf(np.concatenate([ident, ones, sel.reshape(128, 64 * 128)], axis=1))
    for L in (LS, LP):
        s = np.arange(L, dtype=np.float64)[:, None]
        f = np.arange(L, dtype=np.float64)[None, :]
        ang2 = np.pi * (2 * f + 1) * (2 * s + 1) / (4 * L)
        angk = np.pi * (2 * f + 1) * s / (2 * L)
        mats = np.stack([np.cos(ang2), np.sin(ang2), np.cos(angk), np.sin(angk)], 0)
        c["dft%d" % L] = _bf(mats.reshape(4, L // 128, 128, L).transpose(2, 0, 1, 3))
        t = np.linspace(0.0, 1.0, L, dtype=np.float32)[:, None]
        w = (2.0 * np.pi * np.arange(L, dtype=np.float32) / L).astype(np.float32)
        fb = np.linspace(1e-4, 15, 16, dtype=np.float32)
        ang = w[:, None] * fb[None, :]
        z = np.concatenate([t, np.cos(ang), -np.sin(ang)], axis=-1).astype(np.float32)
        c["zT%d" % L] = np.ascontiguousarray(z.T)
        deltas = np.linspace(math.log(1e-2) / 1.5, math.log(1e-2) / 0.3, 512, dtype=np.float32)
        dec = np.exp(-t * np.abs(deltas)[None, :]).astype(np.float32)
        c["dec%d" % L] = np.ascontiguousarray(dec.reshape(L // 128, 128, 512).transpose(1, 0, 2))
    rows = LS // 64
    row = np.repeat(np.arange(rows, dtype=np.float32), 64)
    col = np.tile(np.arange(64, dtype=np.float32), rows)
    q = D // 4
    omega = (1.0 / (10000.0 ** (np.arange(q, dtype=np.float32) / q))).astype(np.float32)
    er = row[:, None] * omega
    ec = col[:, None] * omega
    c["pos"] = np.concatenate([np.sin(er), np.cos(er), np.sin(ec), np.cos(ec)], axis=-1).astype(np.float32)
    return c


def build(stage=99):
    k = KB()
    nc = k.nc
    pe, act, dve, pool, sp = k.pe, k.act, k.dve, k.pool, k.sp
    dbg = {}

    def din(name, shape, dtype=F32):
        return nc.dram_tensor(name, list(shape), dtype, kind="ExternalInput").ap()

    def dout(name, shape, dtype=F32):
        return nc.dram_tensor(name, list(shape), dtype, kind="ExternalOutput").ap()

    xs_d = din("xs", [LS, D])
    xp_d = din("xp", [2 * LP, D])
    pos_d = din("pos", [LS, D])
    vecs_d = din("vecs", [128, 128])
    h0_d = din("h0", [2, 2, G, P])
    cst_d = din("cst", [128, 768])
    cstb_d = din("cstb", [128, 256 + 8192], BF16)
    w_ada_d = din("w_ada", [D, 6 * D], F32R)
    w_in_d = din("w_in", [D, 2048], F32R)
    w_glu_d = din("s5_w_glu", [512, 512], F32R)
    w_out_d = din("w_out", [D, D], F32R)
    wr_d = din("moe_wr", [D, 20], F32R)
    br_d = din("moe_br", [1, 20])
    wgate_d = din("moe_w_gate", [16, D, 512], F32R)
    wup_d = din("moe_w_up", [16, D, 512], F32R)
    wdown_d = din("moe_w_down", [16, 512, D], F32R)
    x1_t = T(nc.dram_tensor("x1_scratch", [NTOK, D], F32, kind="Internal").ap(), "x1_scratch")
    h2_t = T(nc.dram_tensor("h2_scratch", [128, 8, NTOK], F32R, kind="Internal").ap(), "h2_scratch")
    kt_t = {L: T(nc.dram_tensor("kt_scratch%d" % L, [128, (L // 128) * 2 * 2 * 512], BF16, kind="Internal").ap(), "kt_scratch%d" % L)
            for L in (LS, LP)}
    vx_t = T(nc.dram_tensor("vx_scratch", [12, 128, NTOK], BF16, kind="Internal").ap(), "vx_scratch")
    hyw1_d = din("hy_f_w1", [33, 64])
    hyw2_d = din("hy_f_w2", [64, 64])
    hyw3_d = din("hy_f_w3", [64, 2048])
    hyb1_d = din("hy_f_b1", [64, 1])
    hyb2_d = din("hy_f_b2", [64, 1])
    hyfr_d = din("hy_freq", [64, 1])
    hyfb_d = din("hy_fbias", [1, 1024])
    zT_d = {L: din("zT%d" % L, [33, L]) for L in (LS, LP)}
    dec_d = {L: din("dec%d" % L, [128, L // 128, 512]) for L in (LS, LP)}
    dft_d = {L: din("dft%d" % L, [128, 4, L // 128, L], BF16) for L in (LS, LP)}
    y_s = dout("y_s", [LS, D])
    y_p = dout("y_p", [2 * LP, D])

    cst = k.sb("cst", [128, 768], F32)
    k.dma(cst.v, cst_d)
    ident = cst[:, 0:128]
    maskF = cst[:, 128:256]
    maskB = cst[:, 256:384]
    selc = cst[0:2, 384:640]
    ones = cst[:, 640:768]
    cstb = k.sb("cstb", [128, 256], BF16)
    k.dma(cstb.v, cstb_d[:, 0:256])
    ident_b = cstb[:, 0:128]
    ones_b = cstb[:, 128:256]
    selh = {}

    def sel(i):
        return selh["t"][:, i * 128:(i + 1) * 128]

    vecT = k.sb("vecT", [128, 128], F32)
    with k.scope() as stv:
        vrow = k.sb("vrow", [128, 128], F32, stack=stv)
        k.dma(vrow.v, vecs_d)
        pt = k.ps()
        k.tr(pt[:, 0:128], vrow.v, ident)
        k.cp(dve, vecT.v, pt[:, 0:128])
    condS = k.sb("condS", [128, 8, 2], F32R)
    k.actv(condS.v.re("p kc n -> p n kc"), vecT[:, 0:16].re("p (n kc) -> p n kc", n=2), AF.Silu)

    def hyena_filter(L, Ktab):
        nf = L // 128
        with k.scope() as st:
            sbt = lambda n, s, d=F32: k.sb(n, s, d, stack=st)
            w3 = sbt("hw3", [64, 2048]); k.dma(w3.v, hyw3_d)
            fb = sbt("hfb", [128, 2, 512]); k.dma(fb.v.re("p a b -> p (a b)"), hyfb_d.broadcast_to([128, 1024]))
            k.ts(dve, fb.v, fb.v, 1.0 / L, ALU.mult)
            h2 = sbt("fh2", [64, L])
            with k.scope() as stm:
                sbm = lambda n, s, d=F32: k.sb(n, s, d, stack=stm)
                zT = sbm("zT", [33, L])
                k.dma(zT.v, zT_d[L])
                w1 = sbm("hw1", [33, 64]); k.dma(w1.v, hyw1_d)
                w2 = sbm("hw2", [64, 64]); k.dma(w2.v, hyw2_d)
                b1 = sbm("hb1", [64, 1]); k.dma(b1.v, hyb1_d)
                b2 = sbm("hb2", [64, 1]); k.dma(b2.v, hyb2_d)
                fr = sbm("hfr", [64, 1]); k.dma(fr.v, hyfr_d)
                h1 = sbm("fh1", [64, L])
                pre = sbm("fpre", [64, 512]); kk_ = sbm("fkk", [64, 512])

                def layer(hout, hin, W, b):
                    for t0 in range(0, L, 512):
                        n = min(512, L - t0)
                        pl = k.ps()
                        k.mm(pl[0:64, 0:n], W, hin[:, t0:t0 + n])
                        k.ts(dve, pre[:, 0:n], pl[0:64, 0:n], b.v, ALU.add, fr.v, ALU.mult)
                        k.ts(dve, kk_[:, 0:n], pre[:, 0:n], 1.0 / TWO_PI, ALU.mult, MAGIC, ALU.add)
                        k.ts(dve, kk_[:, 0:n], kk_[:, 0:n], MAGIC, ALU.subtract)
                        k.stt(dve, pre[:, 0:n], kk_[:, 0:n], -TWO_PI, pre[:, 0:n], ALU.mult, ALU.add)
                        k.ts(dve, pre[:, 0:n], pre[:, 0:n], 3.14159, ALU.min, -3.14159, ALU.max)
                        k.actv(hout[:, t0:t0 + n], pre[:, 0:n], AF.Sin)

                layer(h1, zT.v, w1.v, b1)
                layer(h2, h1.v, w2.v, b2)
            FK = k.sb("FK", [128, 2, nf, L], BF16, stack=st)
            k.dma(FK.v, dft_d[L][:, 2:4])
            dec = sbt("dec", [128, nf, 512]); k.dma(dec.v, dec_d[L])
            he = k.sb("he", [128, nf, 512], BF16, stack=st)
            ho = k.sb("ho", [128, nf, 512], BF16, stack=st)
            hds = [sbt("hd%d" % i, [128, 2, 512]) for i in range(2)]
            for o in range(2):
                for s_ in range(nf):
                    hd = hds[s_ % 2]
                    pq = [k.ps() for _ in range(2)]
                    for d_ in range(2):
                        q = 2 * o + d_
                        k.mm(pq[d_].v, h2[:, s_ * 128:(s_ + 1) * 128], w3[:, q * 512:(q + 1) * 512])
                    for d_ in range(2):
                        k.tt(dve, hd[:, d_, :], pq[d_].v, dec[:, s_, :], ALU.mult)
                    if s_ == 0:
                        k.memset(dve, hd[0:1, 1, :], 0.0)
                    k.tt(dve, he[:, s_, :], hd[:, 0, :], hd[:, 1, :], ALU.add)
                    k.tt(pool, ho[:, s_, :], hd[:, 0, :], hd[:, 1, :], ALU.subtract)
                for ft in range(nf):
                    pr = k.ps(); pi_ = k.ps()
                    for s_ in range(nf):
                        k.mm(pr.v, FK[:, 0, s_, ft * 128:(ft + 1) * 128], he[:, s_, :], start=(s_ == 0), stop=(s_ == nf - 1))
                    for s_ in range(nf):
                        k.mm(pi_.v, FK[:, 1, s_, ft * 128:(ft + 1) * 128], ho[:, s_, :], start=(s_ == 0), stop=(s_ == nf - 1))
                    k.stt(dve, Ktab[:, ft, 0, o, :], pr.v, 1.0 / L, fb[:, o, :], ALU.mult, ALU.add)
                    k.actv(Ktab[:, ft, 1, o, :], pi_.v, AF.Copy, scale=-1.0 / L)

    modT = k.sb("modT", [128, 48, 2], F32)
    _scm = k.scope()
    st_mod = _scm.__enter__()
    wa = [k.sb("wa%d" % i, [128, 8, 512], F32R, stack=st_mod) for i in range(2)]
    mstg = [k.sb("mstg%d" % i, [2, 512], F32, stack=st_mod) for i in range(2)]
    w_ada_v = w_ada_d.rearrange("(kc p) n -> p kc n", p=128)
    for nb in range(2):
        k.dma(wa[nb].v, w_ada_v[:, :, nb * 512:(nb + 1) * 512])
    k.mark("filters")
    for L_ in (LS, LP):
        with k.scope() as stf:
            Kt0 = k.sb("Kt0", [128, L_ // 128, 2, 2, 512], BF16, stack=stf)
            hyena_filter(L_, Kt0)
            k.dma(kt_t[L_].v, Kt0.v.re("p a b c d -> p (a b c d)"))
    k.mark("mod")

    for nb in range(12):
        wt = wa[nb % 2]
        if nb >= 2:
            k.dma(wt.v, w_ada_v[:, :, nb * 512:(nb + 1) * 512])
        pm = k.ps()
        for kc in range(8):
            k.mm(pm[0:2, :], condS[:, kc, :], wt[:, kc, :], start=(kc == 0), stop=(kc == 7))
        sg = mstg[nb % 2]
        k.cp(act, sg.v, pm[0:2, :])
        pT = k.ps()
        for q in range(4):
            k.tr(pT[:, 2 * q:2 * q + 2], sg[:, q * 128:(q + 1) * 128], ident[0:2, 0:2], inc=(q == 3))
        k.tt(dve, modT[:, 4 * nb:4 * nb + 4, :], pT[:, 0:8].re("p (c n) -> p c n", n=2),
             vecT[:, 80 + 4 * nb:84 + 4 * nb].re("p (c o) -> p c o", o=1).bc([128, 4, 2]), ALU.add)
    k.ts(dve, modT[:, 8:16, :], modT[:, 8:16, :], 1.0, ALU.add)
    k.ts(dve, modT[:, 32:40, :], modT[:, 32:40, :], 1.0, ALU.add)
    _scm.__exit__(None, None, None)
    ln_d = {nm: din(nm, [1, D]) for nm in ("ln1_g", "ln1_b", "ln2_g", "ln2_b")}

    def load_bc(stack, which, conds, lnames):
        gb = {}
        dg = k.sb("dg", [128, 128], F32, stack=stack)
        for cond in conds:
            t = k.sb("gbc%d" % cond, [128, D], F32, stack=stack)
            for hb in range(2):
                pb = k.ps()
                for q in range(4):
                    c = (16 if which == 0 else 40) + hb * 4 + q
                    k.ts(dve, dg.v, ident, modT[:, c, cond:cond + 1], ALU.mult)
                    k.mm(pb[:, q * 128:(q + 1) * 128], ones, dg.v)
                k.cp(act, t[:, hb * 512:(hb + 1) * 512], pb.v)
            gb[cond] = t
        lb = {}
        for nm in lnames:
            t = k.sb("bc_" + nm, [128, D], F32, stack=stack)
            k.dma(t.v, ln_d[nm].broadcast_to([128, D]))
            lb[nm] = t
        return gb, lb

    k.mark("s5prep")
    a_d = [din("s5_a_re", [2, G, P]), din("s5_a_im", [2, G, P])]
    ldt_d = din("s5_log_dt", [2, G])
    b_d = [din("s5_b_re", [2, G, P, H]), din("s5_b_im", [2, G, P, H])]
    c_d = [din("s5_c_re", [G, H, P]), din("s5_c_im", [G, H, P])]
    epsc = k.sb("epsc", [128, 1], F32)
    k.memset(dve, epsc.v, LN_EPS)
    _scB = k.scope()
    stB = _scB.__enter__()
    mixS = k.sb("mixS", [128, 4, NTOK], F32R, stack=stB)
    _scA = k.scope()
    stA = _scA.__enter__()
    selh["t"] = k.sb("selt", [128, 8192], BF16, stack=stA)
    k.dma(selh["t"].v, cstb_d[:, 256:256 + 8192])
    WT = k.sb("WT", [128, G, 2, 128], BF16, stack=stA)
    V2 = k.sb("V2", [128, G, 2, 128], BF16, stack=stA)
    Kmat = k.sb("Kmat", [128, G, 128], BF16, stack=stA)
    mu = k.sb("mu", [128, 2, G], F32, stack=stA)
    PT = k.sb("PT", [128, 2, G, 16], F32, stack=stA)
    mu16 = k.sb("mu16", [128, 3, G], F32, stack=stA)
    F_q, B_q = slice(0, 64), slice(64, 128)
    h0D = k.sb("h0D", [128, 2, G], F32, stack=stA)
    with k.scope() as st:
        sbt = lambda n, s, d=F32: k.sb(n, s, d, stack=st)
        nat = sbt("nat", [32, 4, 128])
        for ri in range(2):
            k.dma(nat[:, ri, :].re("g (d p) -> g d p", d=2), a_d[ri].rearrange("d g p -> g d p"))
            k.dma(nat[:, 2 + ri, :].re("g (d p) -> g d p", d=2), h0_d[ri].rearrange("d g p -> g d p"))
        pa = k.ps()
        for i in range(4):
            k.tr(pa[:, i * 32:(i + 1) * 32], nat[:, i, :], ident[0:32, 0:32], inc=(i == 3))
        aD = sbt("aD", [128, 2, G])
        k.cp(dve, aD.v.re("p a g -> p (a g)"), pa[:, 0:64])
        k.cp(dve, h0D.v.re("p a g -> p (a g)"), pa[:, 64:128])
        ldt = sbt("ldt", [128, G])
        for d_ in range(2):
            k.dma(ldt[d_ * 64:(d_ + 1) * 64, :], ldt_d[d_:d_ + 1, :].broadcast_to([64, G]))
        bD = sbt("bD", [128, 2, G, H])
        for d_ in range(2):
            for ri in range(2):
                k.dma(bD[d_ * 64:(d_ + 1) * 64, ri], b_d[ri][d_].rearrange("g p h -> p g h"))
        cnat = sbt("cnat", [128, 4, 2, 128])
        for ri in range(2):
            for dup in range(2):
                k.dma(cnat[:, :, ri, dup * 64:(dup + 1) * 64], c_d[ri].rearrange("(cc g8) h p -> (g8 h) cc p", cc=4))
        cD = sbt("cD", [128, 2, G, H])
        for ri in range(2):
            pc = k.ps()
            for cc in range(4):
                k.tr(pc[:, cc * 128:(cc + 1) * 128], cnat[:, cc, ri, :], ident, inc=(cc == 3))
            k.cp(act, cD[:, ri].re("p g h -> p (g h)"), pc.v)

        if stage == 13:
            dd = dout("dbg_aD", [128, 2 * G])
            k.dma(dd, aD.v.re("p a g -> p (a g)"), is_output=True)
            dd2 = dout("dbg_cD", [128, 2 * G * H])
            k.dma(dd2, cD.v.re("p a g h -> p (a g h)"), is_output=True)
            dd3 = dout("dbg_bD", [128, 2 * G * H])
            k.dma(dd3, bD.v.re("p a g h -> p (a g h)"), is_output=True)
            dd4 = dout("dbg_ldt", [128, G])
            k.dma(dd4, ldt.v, is_output=True)
            k.finish()
            return k
        tmp = [sbt("ptmp%d" % i, [128, G]) for i in range(8)]

        def horner(out, y, coefs):
            n = len(coefs) - 1
            k.ts(dve, out, y, float(coefs[n]), ALU.mult)
            for kk in range(n - 1, 0, -1):
                k.stt(dve, out, out, float(coefs[kk]), y, ALU.add, ALU.mult)
            k.ts(dve, out, out, float(coefs[0]), ALU.add)

        fact = [1.0 / math.factorial(i) for i in range(12)]

        def exp_acc(out, x, nsq, scratch):
            k.ts(dve, scratch, x, 1.0 / (2 ** nsq), ALU.mult)
            horner(out, scratch, fact[0:12])
            for _ in range(nsq):
                k.tt(dve, out, out, out, ALU.mult)

        dtt = sbt("dtt", [128, G])
        exp_acc(dtt.v, ldt.v, 3, tmp[0].v)
        er = sbt("er", [128, G])
        th = sbt("th", [128, G])
        k.tt(dve, er.v, aD[:, 0, :], dtt.v, ALU.mult)
        k.tt(dve, th.v, aD[:, 1, :], dtt.v, ALU.mult)
        mag = sbt("mag", [128, G])
        exp_acc(mag.v, er.v, 2, tmp[0].v)
        kk_ = tmp[1]
        k.ts(dve, kk_.v, th.v, 1.0 / TWO_PI, ALU.mult, MAGIC, ALU.add)
        k.ts(dve, kk_.v, kk_.v, MAGIC, ALU.subtract)
        C1 = 6.28125
        C2 = TWO_PI - C1
        rq = tmp[2]
        k.stt(dve, rq.v, kk_.v, -C1, th.v, ALU.mult, ALU.add)
        k.stt(dve, rq.v, kk_.v, -C2, rq.v, ALU.mult, ALU.add)
        k.ts(dve, rq.v, rq.v, 0.25, ALU.mult)
        z2 = tmp[3]
        k.tt(dve, z2.v, rq.v, rq.v, ALU.mult)
        sn = sbt("sn", [128, G])
        cs = sbt("cs", [128, G])
        horner(sn.v, z2.v, [1.0, -fact[3], fact[5], -fact[7], fact[9], -fact[11]])
        k.tt(dve, sn.v, sn.v, rq.v, ALU.mult)
        horner(cs.v, z2.v, [1.0, -fact[2], fact[4], -fact[6], fact[8], -fact[10]])
        for _ in range(2):
            k.tt(dve, tmp[4].v, sn.v, cs.v, ALU.mult)
            k.tt(dve, tmp[5].v, sn.v, sn.v, ALU.mult)
            k.ts(dve, sn.v, tmp[4].v, 2.0, ALU.mult)
            k.ts(dve, cs.v, tmp[5].v, -2.0, ALU.mult, 1.0, ALU.add)
        PWp = sbt("PWp", [128, 2, 9, G])
        PWn = sbt("PWn", [128, 2, 8, G])
        k.memset(dve, PWp[:, 0, 0, :], 1.0)
        k.memset(dve, PWp[:, 1, 0, :], 0.0)
        k.memset(dve, PWn[:, 0, 0, :], 1.0)
        k.memset(dve, PWn[:, 1, 0, :], 0.0)
        k.tt(dve, PWp[:, 0, 1, :], mag.v, cs.v, ALU.mult)
        k.tt(dve, PWp[:, 1, 1, :], mag.v, sn.v, ALU.mult)
        ctmp = [sbt("ctmp%d" % i, [128, 8, G]) for i in range(2)]

        def cmul(PW, dst, src, n, mk):
            ar, ai = PW[:, 0, src:src + n, :], PW[:, 1, src:src + n, :]
            br = PW[:, 0, mk:mk + 1, :].bc([128, n, G])
            bi = PW[:, 1, mk:mk + 1, :].bc([128, n, G])
            t0, t1 = ctmp[0][:, 0:n, :], ctmp[1][:, 0:n, :]
            k.tt(dve, t0, ar, br, ALU.mult)
            k.tt(dve, t1, ai, bi, ALU.mult)
            k.tt(dve, PW[:, 0, dst:dst + n, :], t0, t1, ALU.subtract)
            k.tt(dve, t0, ar, bi, ALU.mult)
            k.tt(dve, t1, ai, br, ALU.mult)
            k.tt(dve, PW[:, 1, dst:dst + n, :], t0, t1, ALU.add)

        cmul(PWp, 2, 1, 1, 1)
        cmul(PWp, 3, 1, 2, 2)
        cmul(PWp, 5, 1, 4, 4)
        k.tt(dve, tmp[4].v, PWp[:, 0, 1, :], PWp[:, 0, 1, :], ALU.mult)
        k.tt(dve, tmp[5].v, PWp[:, 1, 1, :], PWp[:, 1, 1, :], ALU.mult)
        k.tt(dve, tmp[4].v, tmp[4].v, tmp[5].v, ALU.add)
        k.recip(tmp[5].v, tmp[4].v)
        k.tt(dve, PWn[:, 0, 1, :], PWp[:, 0, 1, :], tmp[5].v, ALU.mult)
        k.stt(dve, PWn[:, 1, 1, :], PWp[:, 1, 1, :], -1.0, tmp[5].v, ALU.mult, ALU.mult)
        cmul(PWn, 2, 1, 1, 1)
        cmul(PWn, 3, 1, 2, 2)
        cmul(PWn, 5, 1, 3, 4)
        k.cp(dve, mu.v, PWp[:, :, 8, :])
        Qm = sbt("Qm", [128, 2, 16, G])
        k.cp(dve, Qm[:, :, 0, :], PWp[:, :, 8, :])
        cmul(Qm, 1, 0, 1, 0)
        cmul(Qm, 2, 0, 2, 1)
        cmul(Qm, 4, 0, 4, 3)
        cmul(Qm, 8, 0, 8, 7)
        for ri in range(2):
            k.cp(act, PT[F_q, ri], Qm[F_q, ri].re("p k g -> p g k"))
            for kk2 in range(16):
                k.cp(act if kk2 % 2 == 0 else pool, PT[B_q, ri, :, kk2], Qm[B_q, ri, 15 - kk2, :])
        k.cp(dve, mu16[:, 0:2, :], Qm[:, :, 15, :])
        k.ts(dve, mu16[:, 2, :], Qm[:, 1, 15, :], -1.0, ALU.mult)
        if stage == 32:
            for nm, t_ in (("dtt", dtt), ("er", er), ("th", th), ("mag", mag), ("sn", sn), ("cs", cs)):
                k.dma(dout("dbg_" + nm, [128, G]), t_.v, is_output=True)
            k.dma(dout("dbg_PWp", [128, 2 * 9 * G]), PWp.v.re("p a b g -> p (a b g)"), is_output=True)
            k.dma(dout("dbg_PWn", [128, 2 * 8 * G]), PWn.v.re("p a b g -> p (a b g)"), is_output=True)
            k.finish()
            return k
        nr = tmp[0]
        k.ts(dve, nr.v, PWp[:, 0, 1, :], -1.0, ALU.add)
        li = PWp[:, 1, 1, :]
        k.tt(dve, tmp[1].v, aD[:, 0, :], aD[:, 0, :], ALU.mult)
        k.tt(dve, tmp[2].v, aD[:, 1, :], aD[:, 1, :], ALU.mult)
        k.tt(dve, tmp[1].v, tmp[1].v, tmp[2].v, ALU.add)
        k.recip(tmp[2].v, tmp[1].v)
        cfr, cfi = tmp[6], tmp[7]
        k.tt(dve, tmp[3].v, nr.v, aD[:, 0, :], ALU.mult)
        k.tt(dve, tmp[4].v, li, aD[:, 1, :], ALU.mult)
        k.tt(dve, tmp[3].v, tmp[3].v, tmp[4].v, ALU.add)
        k.tt(dve, cfr.v, tmp[3].v, tmp[2].v, ALU.mult)
        k.tt(dve, tmp[3].v, li, aD[:, 0, :], ALU.mult)
        k.tt(dve, tmp[4].v, nr.v, aD[:, 1, :], ALU.mult)
        k.tt(dve, tmp[3].v, tmp[3].v, tmp[4].v, ALU.subtract)
        k.tt(dve, cfi.v, tmp[3].v, tmp[2].v, ALU.mult)
        Bb = sbt("Bb", [128, 2, G, H])
        bt0 = sbt("bt0", [128, G, H])
        bt1 = sbt("bt1", [128, G, H])
        cfr_b = cfr.v.re("p (g o) -> p g o", o=1).bc([128, G, H])
        cfi_b = cfi.v.re("p (g o) -> p g o", o=1).bc([128, G, H])
        k.tt(dve, bt0.v, bD[:, 0], cfr_b, ALU.mult)
        k.tt(dve, bt1.v, bD[:, 1], cfi_b, ALU.mult)
        k.tt(dve, Bb[:, 0], bt0.v, bt1.v, ALU.subtract)
        k.tt(dve, bt0.v, bD[:, 1], cfr_b, ALU.mult)
        k.tt(dve, bt1.v, bD[:, 0], cfi_b, ALU.mult)
        k.tt(dve, Bb[:, 1], bt0.v, bt1.v, ALU.add)
        EW = sbt("EW", [128, 2, G, 8])
        EV = sbt("EV", [128, 2, G, 8])
        EC = sbt("EC", [128, 2, G, 8])
        F_, B_ = slice(0, 64), slice(64, 128)
        for ri in range(2):
            k.cp(act, EW[B_, ri], PWp[B_, ri, 0:8, :].re("p k g -> p g k"))
            k.cp(act, EV[F_, ri], PWp[F_, ri, 1:9, :].re("p k g -> p g k"))
            k.cp(act, EC[B_, ri], PWn[B_, ri, 0:8, :].re("p k g -> p g k"))
            for r in range(8):
                k.cp(act, EW[F_, ri, :, r], PWp[F_, ri, 7 - r, :])
                k.cp(pool, EV[B_, ri, :, r], PWp[B_, ri, 8 - r, :])
                k.cp(act, EC[F_, ri, :, r], PWn[F_, ri, 7 - r, :])
        if stage == 14:
            dd = dout("dbg_mu", [128, 2 * G])
            k.dma(dd, mu.v.re("p a g -> p (a g)"), is_output=True)
            dd2 = dout("dbg_Bb", [128, 2 * G * H])
            k.dma(dd2, Bb.v.re("p a g h -> p (a g h)"), is_output=True)
            dd3 = dout("dbg_EW", [128, 2 * G * 8])
            k.dma(dd3, EW.v.re("p a g r -> p (a g r)"), is_output=True)
            k.finish()
            return k
        GH = G // 2
        big = [sbt("big%d" % i, [128, GH, 8, H]) for i in range(5)]
        sh4 = [128, GH, 8, H]

        def outer(dr, di, E, X, neg_im, gs):
            er_ = E[:, 0, gs].re("p g (r o) -> p g r o", o=1).bc(sh4)
            ei_ = E[:, 1, gs].re("p g (r o) -> p g r o", o=1).bc(sh4)
            xr_ = X[:, 0, gs].re("p g (o h) -> p g o h", o=1).bc(sh4)
            xi_ = X[:, 1, gs].re("p g (o h) -> p g o h", o=1).bc(sh4)
            k.tt(dve, dr.v, er_, xr_, ALU.mult)
            k.tt(pool, di.v, ei_, xi_, ALU.mult)
            k.tt(dve, dr.v, dr.v, di.v, ALU.subtract)
            k.tt(dve, di.v, er_, xi_, ALU.mult)
            k.tt(dve, big[4].v, ei_, xr_, ALU.mult)
            if neg_im:
                k.stt(dve, di.v, di.v, -1.0, big[4].v, ALU.mult, ALU.subtract)
            else:
                k.tt(dve, di.v, di.v, big[4].v, ALU.add)

        Wr, Wi, Cr, Ci = big[0], big[1], big[2], big[3]
        kmA = [sbt("kmA%d" % i, [128, 128]) for i in range(2)]
        kmB = [sbt("kmB%d" % i, [128, 128]) for i in range(2)]
        for gh in range(2):
            gs = slice(gh * GH, (gh + 1) * GH)
            outer(Wr, Wi, EW, Bb, False, gs)
            outer(Cr, Ci, EC, cD, True, gs)
            for gl in range(GH):
                g = gh * GH + gl
                wr = Wr[:, gl].re("p r h -> p (r h)")
                wi = Wi[:, gl].re("p r h -> p (r h)")
                cr = Cr[:, gl].re("p r h -> p (r h)")
                ci = Ci[:, gl].re("p r h -> p (r h)")
                pk = k.ps()
                pk2 = k.ps()
                k.mm(pk[:, 0:128], wr[F_], cr[F_], start=True, stop=False)
                k.mm(pk[:, 0:128], wi[F_], ci[F_], start=False, stop=True)
                k.mm(pk2[:, 0:128], wr[B_], cr[B_], start=True, stop=False)
                k.mm(pk2[:, 0:128], wi[B_], ci[B_], start=False, stop=True)
                kA, kB = kmA[gl % 2], kmB[gl % 2]
                k.tt(dve, kA.v, pk[:, 0:128], maskF, ALU.mult)
                k.tt(dve, kB.v, pk2[:, 0:128], maskB, ALU.mult)
                k.tt(pool, Kmat[:, g, :], kA.v, kB.v, ALU.add)
            for gl in range(GH):
                g = gh * GH + gl
                pw_ = k.ps()
                k.tr(pw_[:, 0:128], Wr[:, gl].re("p r h -> p (r h)"), ident, inc=False)
                k.tr(pw_[:, 128:256], Wi[:, gl].re("p r h -> p (r h)"), ident)
                k.cp(act, WT[:, g].re("p a b -> p (a b)"), pw_[:, 0:256])
            outer(Cr, Ci, EV, cD, True, gs)
            k.cp(act, V2[:, gs, 0, :], Cr.v.re("p g r h -> p g (r h)"))
            k.cp(pool, V2[:, gs, 1, :], Ci.v.re("p g r h -> p g (r h)"))
    if stage <= 2:
        dW = dout("dbg_WT", [128, G * 256], BF16)
        dV = dout("dbg_V2", [128, G * 256], BF16)
        dK = dout("dbg_K", [128, G * 128], BF16)
        dmu = dout("dbg_mu", [128, 2 * G])
        k.dma(dW, WT.v.re("p g a b -> p (g a b)"), is_output=True)
        k.dma(dV, V2.v.re("p g a b -> p (g a b)"), is_output=True)
        k.dma(dK, Kmat.v.re("p g b -> p (g b)"), is_output=True)
        k.dma(dmu, mu.v.re("p a g -> p (a g)"), is_output=True)
        k.finish()
        return k


    st_re_d = dout("st_re", [2, 2, G, P])
    st_im_d = dout("st_im", [2, 2, G, P])
    mu2 = k.sb("mu2", [128, 2, G], F32, stack=stA)
    k.ts(dve, mu2[:, 0, :], mu[:, 1, :], -1.0, ALU.mult)
    k.cp(dve, mu2[:, 1, :], mu[:, 1, :])
    F_, B_ = slice(0, 64), slice(64, 128)

    def norm_mod_T(xt, xn, stt_, mv, rs, cond, moff, dst):
        for hh in range(2):
            k.emit(dve, lambda hh=hh: nc.vector.bn_stats(out=stt_[:, hh, :].ap, in_=xt[:, hh * 512:(hh + 1) * 512].ap),
                   [xt.v], [stt_.v])
        k.emit(dve, lambda: nc.vector.bn_aggr(out=mv.v.ap, in_=stt_.v.re("p a b -> p (a b)").ap), [stt_.v], [mv.v])
        k.ts(dve, rs.v, mv[:, 1:2], LN_EPS, ALU.add)
        k.actv(rs.v, rs.v, AF.Sqrt)
        k.recip(rs.v, rs.v)
        k.ts(dve, xn.v, xt.v, mv[:, 0:1], ALU.subtract, rs.v, ALU.mult)
        for hb in range(2):
            pp = k.ps()
            for q in range(4):
                kc = hb * 4 + q
                k.tr(pp[:, q * 128:(q + 1) * 128], xn[:, kc * 128:(kc + 1) * 128], ident, inc=(q == 3))
            for q in range(4):
                kc = hb * 4 + q
                k.actv(dst(kc), pp[:, q * 128:(q + 1) * 128], AF.Identity,
                       scale=modT[:, moff + 8 + kc, cond:cond + 1], bias=modT[:, moff + kc, cond:cond + 1])

    def ln_front(x_rows, L, cond, h_fm, stack, add_pos=None, moff=0, cond_fn=None):
        xts = [k.sb("xt%d" % i, [128, D], F32, stack=stack) for i in range(2)]
        xns = [k.sb("xn%d" % i, [128, D], F32, stack=stack) for i in range(2)]
        stts = [k.sb("bnst%d" % i, [128, 2, 6], F32, stack=stack) for i in range(2)]
        mvs = [k.sb("mv%d" % i, [128, 2], F32, stack=stack) for i in range(2)]
        rss = [k.sb("rs%d" % i, [128, 1], F32, stack=stack) for i in range(2)]
        for tt in range(L // 128):
            xt, xn, stt_, mv, rs = xts[tt % 2], xns[tt % 2], stts[tt % 2], mvs[tt % 2], rss[tt % 2]
            k.dma(xt.v, x_rows[tt * 128:(tt + 1) * 128, :])
            if cond_fn is not None:
                cond = cond_fn(tt)
            if add_pos is not None:
                k.dma(xn.v, add_pos[tt * 128:(tt + 1) * 128, :])
                k.tt(dve, xt.v, xt.v, xn.v, ALU.add)
            hb_ = h_fm[(tt * 128) // 512]
            o_ = (tt * 128) % 512
            norm_mod_T(xt, xn, stt_, mv, rs, cond, moff, lambda kc, hb_=hb_, o_=o_: hb_[:, kc, o_:o_ + 128])

    def s5_front(u_bf, L, nseq, XsFB, U8):
        XsF, XsB = XsFB
        J = L // 8
        NJ = nseq * J
        gpb = 512 // NJ
        for g0 in range(0, G, gpb):
            pb = k.ps()
            for gi in range(gpb):
                g = g0 + gi
                cc, g8 = g // 8, g % 8
                uv = u_bf[:, cc, :].re("p (j r) -> p r j", r=8)
                for r in range(8):
                    k.mm(pb[:, gi * NJ:(gi + 1) * NJ], sel(g8 * 8 + r), uv[:, r, :], start=(r == 0), stop=(r == 7),
                         inc=(r == 7 and gi == gpb - 1))
            k.cp(act, U8[:, g0:g0 + gpb, :].re("p g j -> p (g j)"), pb[:, 0:gpb * NJ])
        for ri in range(2):
            for g0 in range(0, G, gpb):
                pb = k.ps()
                for gi in range(gpb):
                    g = g0 + gi
                    k.mm(pb[:, gi * NJ:(gi + 1) * NJ], WT[:, g, ri, :], U8[:, g, :], inc=(gi == gpb - 1))
                for s in range(nseq):
                    src = pb[:, 0:gpb * NJ].re("p (g s j) -> p g s j", s=nseq, j=J)
                    k.cp(act, XsF[F_, ri, g0:g0 + gpb, s, 1:J + 1], src[F_, :, s, :])
                    k.cp(dve, XsB[B_, ri, g0:g0 + gpb, s, 0:J], src[B_, :, s, :])

    w_in_v = w_in_d.rearrange("(kc p) n -> p kc n", p=128)

    def proj_front(h_fm, L, nseq, stack, u_bf, vx1, x2, blocks, tok0=0):
        NT = nseq * L
        wb = [k.sb("wblk%d" % i, [128, 8, 512], F32R, stack=stack) for i in range(2)]
        pads = [k.sb("pad%d" % i, [128, nseq, L + 2], F32, stack=stack) for i in range(2 if 0 in blocks else 0)]
        acc = [k.sb("cacc%d" % i, [128, nseq, L], F32, stack=stack) for i in range(1 if 0 in blocks else 0)]
        stg = [k.sb("cstg%d" % i, [128, nseq, L], BF16, stack=stack) for i in range(2 if 0 in blocks else 0)]
        for pd in pads:
            k.memset(pool, pd[:, :, 0:1], 0.0)
            k.memset(pool, pd[:, :, L + 1:L + 2], 0.0)
        ci = 0
        for bi, b in enumerate(blocks):
            wt = wb[bi % 2]
            k.dma(wt.v, w_in_v[:, :, b * 512:(b + 1) * 512])
            for q in range(4):
                ch = b * 4 + q
                pbs = []
                for t0 in range(0, NT, 512):
                    n = min(512, NT - t0)
                    pu = k.ps()
                    for kc in range(8):
                        k.mm(pu[:, 0:n], wt[:, kc, q * 128:(q + 1) * 128], h_fm[t0 // 512][:, kc, 0:n], start=(kc == 0), stop=(kc == 7))
                    pbs.append((pu, t0, n))
                if b == 3:
                    for pu, t0, n in pbs:
                        k.cp(act, u_bf[:, q, t0:t0 + n], pu[:, 0:n])
                    continue
                pd, ac = pads[ci % 2], acc[0]
                ci += 1
                for pu, t0, n in pbs:
                    if L >= 512:
                        s = t0 // L
                        l0 = t0 % L
                        k.cp(act, pd[:, s, 1 + l0:1 + l0 + n], pu[:, 0:n])
                    else:
                        k.cp(act, pd[:, t0 // L:(t0 + n) // L, 1:L + 1], pu[:, 0:n].re("p (s l) -> p s l", l=L))
                eng = dve
                w0 = vecT[:, 24 + ch:25 + ch]
                w1 = vecT[:, 36 + ch:37 + ch]
                w2 = vecT[:, 48 + ch:49 + ch]
                bb = vecT[:, 60 + ch:61 + ch]
                k.ts(eng, ac.v, pd[:, :, 0:L], w0, ALU.mult, bb, ALU.add)
                k.stt(dve, ac.v, pd[:, :, 1:L + 1], w1, ac.v, ALU.mult, ALU.add)
                sg = stg[ci % 2]
                k.stt(dve, sg.v, pd[:, :, 2:L + 2], w2, ac.v, ALU.mult, ALU.add)
                k.dma(vx_t[ch, :, tok0:tok0 + NT], sg.v.re("p s l -> p (s l)"))

    def s5_scan_seq(XsFB, J, nseq, stack):
        tA = [k.sb("scA%d" % i, [128, 2, G, nseq], F32, stack=stack) for i in range(2)]
        tB = [k.sb("scB%d" % i, [128, 2, G, nseq], F32, stack=stack) for i in range(2)]
        shp = [64, 2, G, nseq]
        for j in range(J):
            for di, (eng, half) in enumerate(((dve, F_), (dve, B_))):
                Xs = XsFB[di]
                prev = j if di == 0 else J - j
                cur = j + 1 if di == 0 else J - 1 - j
                Sp = Xs[half, :, :, :, prev]
                m1 = mu[half, 0:1, :].re("p a (g o) -> p a g o", o=1).bc(shp)
                a, b = tA[di], tB[di]
                k.tt(eng, a[half], Sp, m1, ALU.mult)
                k.tt(eng, b[half, 0], Xs[half, 1, :, :, prev], mu2[half, 0, :].re("p (g o) -> p g o", o=1).bc([64, G, nseq]), ALU.mult)
                k.tt(eng, b[half, 1], Xs[half, 0, :, :, prev], mu2[half, 1, :].re("p (g o) -> p g o", o=1).bc([64, G, nseq]), ALU.mult)
                k.tt(eng, a[half], a[half], b[half], ALU.add)
                k.tt(eng, Xs[half, :, :, :, cur], Xs[half, :, :, :, cur], a[half], ALU.add)


    def s5_scan(XsFB, J, nseq, stack):
        Jg = 16
        nseg = J // Jg
        tA0 = k.sb("scA", [128, 2, G, nseg], F32, stack=stack)
        tB0 = k.sb("scB", [128, 2, G, nseg], F32, stack=stack)
        Cc0 = k.sb("scC", [128, 2, G, nseg], F32, stack=stack)
        u10 = k.sb("scU1", [128, G, nseg, Jg], F32, stack=stack)
        u20 = k.sb("scU2", [128, G, nseg, Jg], F32, stack=stack)
        tA = (tA0, k.alias(tA0, "scAb", stack)); tB = (tB0, k.alias(tB0, "scBb", stack))
        Cc = (Cc0, k.alias(Cc0, "scCb", stack)); u1 = (u10, k.alias(u10, "scU1b", stack)); u2 = (u20, k.alias(u20, "scU2b", stack))
        for s in range(nseq):
            for di, (eng, half) in enumerate(((dve, F_), (dve, B_))):
                Xs = XsFB[di]
                c0 = 1 if di == 0 else 0
                Xv = Xs[half, :, :, s, c0:c0 + J].re("p a g (q k) -> p a g q k", k=Jg)
                a, b, C, w1, w2 = tA[di], tB[di], Cc[di], u1[di], u2[di]
                m1 = mu[half, 0:1, :].re("p a (g o) -> p a g o", o=1).bc([64, 2, G, nseg])
                m2a = mu2[half, 0, :].re("p (g o) -> p g o", o=1).bc([64, G, nseg])
                m2b = mu2[half, 1, :].re("p (g o) -> p g o", o=1).bc([64, G, nseg])
                for step in range(1, Jg):
                    kc_ = step if di == 0 else Jg - 1 - step
                    kp_ = kc_ - 1 if di == 0 else kc_ + 1
                    k.tt(eng, a[half], Xv[:, :, :, :, kp_], m1, ALU.mult)
                    k.tt(eng, b[half, 0], Xv[:, 1, :, :, kp_], m2a, ALU.mult)
                    k.tt(eng, b[half, 1], Xv[:, 0, :, :, kp_], m2b, ALU.mult)
                    k.tt(eng, a[half], a[half], b[half], ALU.add)
                    k.tt(eng, Xv[:, :, :, :, kc_], Xv[:, :, :, :, kc_], a[half], ALU.add)
                h0col = Xs[half, :, :, s, 0] if di == 0 else Xs[half, :, :, s, J]
                order = list(range(nseg)) if di == 0 else list(range(nseg - 1, -1, -1))
                k.cp(eng, C[half, :, :, order[0]], h0col)
                kend = Jg - 1 if di == 0 else 0
                r16 = mu16[half, 0:1, :].bc([64, 2, G])
                for ii in range(nseg - 1):
                    sg, nx = order[ii], order[ii + 1]
                    Cs = C[half, :, :, sg]
                    k.tt(eng, a[half, :, :, 0], Cs, r16, ALU.mult)
                    k.tt(eng, b[half, 0, :, 0], C[half, 1, :, sg], mu16[half, 2, :], ALU.mult)
                    k.tt(eng, b[half, 1, :, 0], C[half, 0, :, sg], mu16[half, 1, :], ALU.mult)
                    k.tt(eng, a[half, :, :, 0], a[half, :, :, 0], b[half, :, :, 0], ALU.add)
                    k.tt(eng, C[half, :, :, nx], Xv[:, :, :, sg, kend], a[half, :, :, 0], ALU.add)
                sh = [64, G, nseg, Jg]
                pr = PT[half, 0].re("p g (o k) -> p g o k", o=1).bc(sh)
                pi_ = PT[half, 1].re("p g (o k) -> p g o k", o=1).bc(sh)
                cr = C[half, 0].re("p g (q o) -> p g q o", o=1).bc(sh)
                ci = C[half, 1].re("p g (q o) -> p g q o", o=1).bc(sh)
                k.tt(eng, w1[half], pr, cr, ALU.mult)
                k.tt(eng, w2[half], pi_, ci, ALU.mult)
                k.tt(eng, w1[half], w1[half], w2[half], ALU.subtract)
                k.tt(eng, Xv[:, 0], Xv[:, 0], w1[half], ALU.add)
                k.tt(eng, w1[half], pr, ci, ALU.mult)
                k.tt(eng, w2[half], pi_, cr, ALU.mult)
                k.tt(eng, w1[half], w1[half], w2[half], ALU.add)
                k.tt(eng, Xv[:, 1], Xv[:, 1], w1[half], ALU.add)

    wglu = k.sb("wglu", [128, 4, 512], F32R, stack=stA)
    k.dma(wglu.v, w_glu_d.rearrange("(kc p) n -> p kc n", p=128))
    GC = 2.0 * math.sqrt(2.0 / math.pi)

    def s5_back(XsFB, U8, u_bf, L, nseq, stack, y_fm):
        XsF, XsB = XsFB
        J = L // 8
        NJ = nseq * J
        gpb = 512 // NJ
        Xprev = k.sb("Xprev", [128, 2, G, nseq, J], BF16, stack=stack)
        for ri in range(2):
            k.cp(dve, Xprev[F_, ri], XsF[F_, ri, :, :, 0:J])
            k.cp(act, Xprev[B_, ri], XsB[B_, ri, :, :, 1:J + 1])
        y8 = k.sb("y8", [128, G, NJ], BF16, stack=stack)
        for g0 in range(0, G, gpb):
            pb = k.ps()
            for gi in range(gpb):
                g = g0 + gi
                o = pb[:, gi * NJ:(gi + 1) * NJ]
                k.mm(o, Kmat[:, g, :], U8[:, g, :], start=True, stop=False)
                k.mm(o, V2[:, g, 0, :], Xprev[:, 0, g].re("p s j -> p (s j)"), start=False, stop=False)
                k.mm(o, V2[:, g, 1, :], Xprev[:, 1, g].re("p s j -> p (s j)"), start=False, stop=True, inc=(gi == gpb - 1))
            k.cp(act, y8[:, g0:g0 + gpb, :].re("p g j -> p (g j)"), pb[:, 0:gpb * NJ])
        rpb = min(8, 512 // NJ)
        for cc in range(4):
            yv = y_fm[:, cc, :].re("p (sj r) -> p r sj", r=8)
            uv = u_bf[:, cc, :].re("p (sj r) -> p r sj", r=8)
            for r0 in range(0, 8, rpb):
                pb = k.ps()
                for rr in range(rpb):
                    r = r0 + rr
                    for g8 in range(8):
                        k.mm(pb[:, rr * NJ:(rr + 1) * NJ], sel(r * 8 + g8), y8[:, cc * 8 + g8, :], start=(g8 == 0), stop=(g8 == 7),
                             inc=(g8 == 7 and rr == rpb - 1))
                for rr in range(rpb):
                    r = r0 + rr
                    k.stt(dve, yv[:, r, :], uv[:, r, :], vecT[:, 72 + cc:73 + cc], pb[:, rr * NJ:(rr + 1) * NJ], ALU.mult, ALU.add)

    def glu_rms(y_fm, NT, stack, mixed_fm, moff, gcol):
        y_r = k.sb("y_r", [128, 4, NT], F32R, stack=stack)
        k.cp(act, y_r.v, y_fm.v)
        ys5 = k.sb("ys5", [128, 4, NT], F32, stack=stack)
        sg = [k.sb("sg%d" % i, [128, 512], F32, stack=stack) for i in range(2)]
        ge = k.sb("ge", [128, 4, NT], F32, stack=stack)
        k.tt(dve, ge.v, y_fm.v, y_fm.v, ALU.mult)
        k.ts(dve, ge.v, ge.v, 0.044715, ALU.mult, 1.0, ALU.add)
        k.tt(dve, ge.v, ge.v, y_fm.v, ALU.mult)
        k.actv(ge.v, ge.v, AF.Sigmoid, scale=GC)
        k.tt(dve, ge.v, ge.v, y_fm.v, ALU.mult)
        bi = 0
        for nch in range(4):
            for t0 in range(0, NT, 512):
                n = min(512, NT - t0)
                pg = k.ps()
                for cc in range(4):
                    k.mm(pg[:, 0:n], wglu[:, cc, nch * 128:(nch + 1) * 128], y_r[:, cc, t0:t0 + n], start=(cc == 0), stop=(cc == 3))
                s_ = sg[bi % 2]
                bi += 1
                k.actv(s_[:, 0:n], pg[:, 0:n], AF.Sigmoid, bias=vecT[:, 76 + nch:77 + nch])
                k.tt(dve, ys5[:, nch, t0:t0 + n], ge[:, nch, t0:t0 + n], s_[:, 0:n], ALU.mult)
        rms_mix(ys5, NT, stack, mixed_fm, moff, gcol)
        return ys5

    def rms_mix(yy, NT, stack, mixed_fm, moff, gcol):
        sq = k.sb("sq", [128, 4, NT], BF16, stack=stack)
        k.actv(sq.v, yy if isinstance(yy, V) else yy.v, AF.Square)
        rbc = k.sb("rbc", [128, 512], F32, stack=stack)
        for t0 in range(0, NT, 512):
            n = min(512, NT - t0)
            pr = k.ps()
            for cc in range(4):
                k.mm(pr[:, 0:n], ones_b, sq[:, cc, t0:t0 + n], start=(cc == 0), stop=(cc == 3))
            k.actv(rbc[:, 0:n], pr[:, 0:n], AF.Sqrt, scale=1.0 / 512.0, bias=epsc.v)
            k.recip(rbc[:, 0:n], rbc[:, 0:n])
            for cc in range(4):
                k.stt(dve, mixed_fm[:, moff + cc, t0:t0 + n], yy[:, cc, t0:t0 + n], vecT[:, gcol + cc:gcol + cc + 1],
                      rbc[:, 0:n], ALU.mult, ALU.mult)


    def to_tm(dst, srcv, L, s):
        for tt in range(L // 128):
            pb = k.ps()
            pbb = pb.v.bitcast(BF16)
            for cc in range(4):
                k.tr(pbb[:, cc * 128:(cc + 1) * 128], srcv[:, cc, s * L + tt * 128: s * L + (tt + 1) * 128], ident_b, inc=(cc == 3))
            k.cp(act, dst[:, tt, :], pbb[:, 0:512])

    def hyena_conv(L, nseq, Ktab, DF, vtm_all, x1tm_all, x2, stack, yhy):
        nf = L // 128
        Ysp = k.sb("Ysp", [128, nf, 2, 512], BF16, stack=stack)
        ta = [k.sb("hta%d" % i, [128, 512], F32, stack=stack) for i in range(4)]

        def fwd_mul(src_tm, o):
            for ft in range(nf):
                pU = k.ps(); pV = k.ps()
                for s_ in range(nf):
                    k.mm(pU.v, DF[:, 0, s_, ft * 128:(ft + 1) * 128], src_tm[:, s_, :], start=(s_ == 0), stop=(s_ == nf - 1))
                for s_ in range(nf):
                    k.mm(pV.v, DF[:, 1, s_, ft * 128:(ft + 1) * 128], src_tm[:, s_, :], start=(s_ == 0), stop=(s_ == nf - 1))
                KA = Ktab[:, ft, 0, o, :]
                KB_ = Ktab[:, ft, 1, o, :]
                k.tt(dve, ta[0].v, pU.v, KA, ALU.mult)
                k.tt(dve, ta[1].v, pV.v, KB_, ALU.mult)
                k.tt(dve, Ysp[:, ft, 0, :], ta[0].v, ta[1].v, ALU.add)
                k.tt(dve, ta[2].v, pV.v, KA, ALU.mult)
                k.tt(dve, ta[3].v, pU.v, KB_, ALU.mult)
                k.tt(pool, Ysp[:, ft, 1, :], ta[2].v, ta[3].v, ALU.subtract)

        for s in range(nseq):
            v_tm = vtm_all[:, s]
            x1_tm = x1tm_all[:, s]
            z_tm = v_tm
            fwd_mul(v_tm, 0)
            for tt in range(nf):
                pz = k.ps()
                for ft in range(nf):
                    k.mm(pz.v, DF[:, 0, ft, tt * 128:(tt + 1) * 128], Ysp[:, ft, 0, :], start=(ft == 0), stop=False)
                    k.mm(pz.v, DF[:, 1, ft, tt * 128:(tt + 1) * 128], Ysp[:, ft, 1, :], start=False, stop=(ft == nf - 1))
                k.tt(dve, z_tm[:, tt, :], pz.v, x1_tm[:, tt, :], ALU.mult)
            fwd_mul(z_tm, 1)
            for cc in range(4):
                for t0 in range(0, L, 512):
                    n = min(512, L - t0)
                    py = k.ps()
                    for ft in range(nf):
                        k.mm(py[:, 0:n], Ysp[:, ft, 0, cc * 128:(cc + 1) * 128], DF[:, 0, ft, t0:t0 + n], start=(ft == 0), stop=False)
                        k.mm(py[:, 0:n], Ysp[:, ft, 1, cc * 128:(cc + 1) * 128], DF[:, 1, ft, t0:t0 + n], start=False, stop=(ft == nf - 1))
                    k.tt(dve, yhy[:, cc, s * L + t0: s * L + t0 + n], py[:, 0:n], x2[:, cc, s * L + t0: s * L + t0 + n], ALU.mult)


    def ln_tile(r_, stack_tiles, gk, bk, out_v):
        stt_, mv, rs, xn = stack_tiles
        for hh in range(2):
            k.emit(dve, lambda hh=hh: nc.vector.bn_stats(out=stt_[:, hh, :].ap, in_=r_[:, hh * 512:(hh + 1) * 512].ap), [r_], [stt_.v])
        k.emit(dve, lambda: nc.vector.bn_aggr(out=mv.v.ap, in_=stt_.v.re("p a b -> p (a b)").ap), [stt_.v], [mv.v])
        k.ts(dve, rs.v, mv[:, 1:2], LN_EPS, ALU.add)
        k.actv(rs.v, rs.v, AF.Sqrt)
        k.recip(rs.v, rs.v)
        k.ts(dve, xn.v, r_, mv[:, 0:1], ALU.subtract, rs.v, ALU.mult)
        k.tt(dve, xn.v, xn.v, gk.v, ALU.mult)
        k.tt(pool, out_v, xn.v, bk.v, ALU.add)

    def out_proj_ln1(mixH_, mixS_, x_rows, NT, cond, stack, row0, add_pos=None):
        wo = k.sb("wo", [128, 8, D], F32R, stack=stack)
        k.dma(wo.v, w_out_d.rearrange("(kc p) n -> p kc n", p=128))
        xts = [k.sb("oxt%d" % i, [128, D], F32, stack=stack) for i in range(2)]
        tmps = [k.sb("otmp%d" % i, [128, D], F32, stack=stack) for i in range(2)]
        x1o = [k.sb("x1o%d" % i, [128, D], F32, stack=stack) for i in range(2)]
        h2stg = [k.sb("h2stg%d" % i, [128, 8, 128], F32R, stack=stack) for i in range(2)]
        tl = [(k.sb("obn%d" % i, [128, 2, 6], F32, stack=stack), k.sb("omv%d" % i, [128, 2], F32, stack=stack),
               k.sb("ors%d" % i, [128, 1], F32, stack=stack), k.sb("oxn%d" % i, [128, D], F32, stack=stack)) for i in range(2)]
        tl2 = [(k.sb("pbn%d" % i, [128, 2, 6], F32, stack=stack), k.sb("pmv%d" % i, [128, 2], F32, stack=stack),
                k.sb("prs%d" % i, [128, 1], F32, stack=stack)) for i in range(2)]
        gb, lb = load_bc(stack, 0, [cond], ("ln1_g", "ln1_b"))
        ntl = NT // 128

        def stage1(tt):
            xt, tmp, tiles = xts[tt % 2], tmps[tt % 2], tl[tt % 2]
            k.dma(xt.v, x_rows[tt * 128:(tt + 1) * 128, :])
            if add_pos is not None:
                k.dma(tmp.v, add_pos[tt * 128:(tt + 1) * 128, :])
                k.tt(dve, xt.v, xt.v, tmp.v, ALU.add)
            pos_ = []
            for hb in range(2):
                po = k.ps()
                for kc in range(8):
                    lh = mixH_[:, kc, tt * 128:(tt + 1) * 128] if kc < 4 else mixS_[:, kc - 4, tt * 128:(tt + 1) * 128]
                    k.mm(po.v, lh, wo[:, kc, hb * 512:(hb + 1) * 512], start=(kc == 0), stop=(kc == 7))
                pos_.append(po)
            for hb in range(2):
                k.tt(dve, tmp[:, hb * 512:(hb + 1) * 512], pos_[hb].v, gb[cond][:, hb * 512:(hb + 1) * 512], ALU.mult)
            k.stt(dve, tmp.v, xt.v, ALPHA, tmp.v, ALU.mult, ALU.add)
            xo_ = x1o[tt % 2]
            ln_tile(tmp.v, tiles, lb["ln1_g"], lb["ln1_b"], xo_.v)
            k.dma(x1_t[row0 + tt * 128: row0 + (tt + 1) * 128, :], xo_.v)

        def stage2(tt):
            tmp, t2 = tmps[tt % 2], tl2[tt % 2]
            xo_ = x1o[tt % 2]
            h2s = h2stg[tt % 2]
            norm_mod_T(xo_, tmp, t2[0], t2[1], t2[2], cond, 24, lambda kc, h2s=h2s: h2s[:, kc, :])
            k.dma(h2_t[:, :, row0 + tt * 128: row0 + (tt + 1) * 128], h2s.v)

        for tt in range(ntl + 1):
            if tt < ntl:
                stage1(tt)
            if tt >= 1:
                stage2(tt - 1)

    BIG = 1.0e30

    def moe_half(hh, stack):
        NTH = NTOK
        NTL = NTH // 128
        TB = 512
        h2 = k.sb("h2", [128, 8, NTH], F32R, stack=stack)
        k.dma(h2.v, h2_t[:, :, hh * NTH:(hh + 1) * NTH])
        wr = k.sb("wr", [128, 8, 20], F32R, stack=stack)
        k.dma(wr.v, wr_d.rearrange("(kc p) n -> p kc n", p=128))
        brb = k.sb("brb", [128, 20], F32, stack=stack)
        k.dma(brb.v, br_d.broadcast_to([128, 20]))
        gates = k.sb("gates", [128, NTL, 16], F32, stack=stack)
        with k.scope() as st3:
            sm = lambda n, s: k.sb(n, s, F32, stack=st3)
            T_ = NTL
            lg = sm("lg", [128, T_, 20]); m1 = sm("m1", [128, T_]); d1 = sm("d1", [128, T_, 4]); e1 = sm("e1", [128, T_, 4])
            s1 = sm("s1", [128, T_]); pg = sm("pg", [128, T_]); oh = sm("oh", [128, T_, 4]); l2m = sm("l2m", [128, T_, 16])
            mx1 = sm("mx1", [128, T_]); oh1 = sm("oh1", [128, T_, 16]); l2b = sm("l2b", [128, T_, 16]); mx2 = sm("mx2", [128, T_])
            oh2 = sm("oh2", [128, T_, 16]); dd = sm("dd", [128, T_]); w1_ = sm("w1_", [128, T_]); w2_ = sm("w2_", [128, T_])
            gt = sm("gt", [128, T_, 16])
            pl = k.ps()
            for tt in range(T_):
                for kc in range(8):
                    k.mm(pl[:, tt * 20:(tt + 1) * 20], h2[:, kc, tt * 128:(tt + 1) * 128], wr[:, kc, :], start=(kc == 0), stop=(kc == 7),
                         inc=(kc == 7 and tt == T_ - 1))
            bcl = lambda v_, n_: v_.re("p (t o) -> p t o", o=1).bc([128, T_, n_])
            k.tt(dve, lg.v, pl[:, 0:T_ * 20].re("p (t c) -> p t c", c=20), brb.v.re("p (o c) -> p o c", o=1).bc([128, T_, 20]), ALU.add)
            k.reduce(m1.v, lg[:, :, 0:4], ALU.max)
            k.tt(dve, d1.v, lg[:, :, 0:4], bcl(m1.v, 4), ALU.subtract)
            k.actv(e1.v, d1.v, AF.Exp)
            k.reduce(s1.v, e1.v, ALU.add)
            k.recip(pg.v, s1.v)
            k.ts(dve, oh.v, d1.v, 0.0, ALU.is_equal)
            k.ts(dve, oh.v, oh.v, BIG, ALU.mult, -BIG, ALU.add)
            for g_i in range(4):
                k.tt(dve, l2m[:, :, g_i * 4:(g_i + 1) * 4], lg[:, :, 4 + g_i * 4:8 + g_i * 4],
                     oh[:, :, g_i:g_i + 1].bc([128, T_, 4]), ALU.add)
            k.reduce(mx1.v, l2m.v, ALU.max)
            k.tt(dve, oh1.v, l2m.v, bcl(mx1.v, 16), ALU.is_equal)
            k.stt(dve, l2b.v.re("p t c -> p (t c)"), oh1.v.re("p t c -> p (t c)"), -BIG, l2m.v.re("p t c -> p (t c)"), ALU.mult, ALU.add)
            k.reduce(mx2.v, l2b.v, ALU.max)
            k.tt(dve, oh2.v, l2b.v, bcl(mx2.v, 16), ALU.is_equal)
            k.tt(dve, dd.v, mx2.v, mx1.v, ALU.subtract)
            k.actv(dd.v, dd.v, AF.Exp)
            k.ts(dve, w1_.v, dd.v, 1.0, ALU.add)
            k.recip(w1_.v, w1_.v)
            k.tt(dve, w2_.v, dd.v, w1_.v, ALU.mult)
            k.tt(dve, w1_.v, w1_.v, pg.v, ALU.mult)
            k.tt(dve, w2_.v, w2_.v, pg.v, ALU.mult)
            k.tt(dve, gt.v, oh1.v, bcl(w1_.v, 16), ALU.mult)
            k.tt(dve, oh2.v, oh2.v, bcl(w2_.v, 16), ALU.mult)
            k.tt(dve, gates.v, gt.v, oh2.v, ALU.add)
        y_acc = k.sb("y_acc", [128, NTL, D], F32, stack=stack)
        _scw = k.scope()
        stack = _scw.__enter__()
        wg = [k.sb("wg%d" % i, [128, 8, 512], F32R, stack=stack) for i in range(2)]
        wu = [k.sb("wu%d" % i, [128, 8, 512], F32R, stack=stack) for i in range(2)]
        wd = [k.sb("wd%d" % i, [128, 4, D], F32R, stack=stack) for i in range(1)]
        hids = [k.sb("hid%d" % i, [128, 4, TB], F32R, stack=stack) for i in range(2)]
        sa = [k.sb("sa%d" % i, [128, TB], F32, stack=stack) for i in range(2)]
        for e in range(16):
            g_, u_, d_ = wg[e % 2], wu[e % 2], wd[0]
            k.dma(g_.v, wgate_d[e].rearrange("(kc p) n -> p kc n", p=128))
            k.dma(u_.v, wup_d[e].rearrange("(kc p) n -> p kc n", p=128))
            k.dma(d_.v, wdown_d[e].rearrange("(kc p) n -> p kc n", p=128))
            def gu(tb):
                hd_ = hids[(tb // TB) % 2]
                for fc in range(4):
                    pa = k.ps(); pu = k.ps()
                    for kc in range(8):
                        k.mm(pa[:, 0:TB], g_[:, kc, fc * 128:(fc + 1) * 128], h2[:, kc, tb:tb + TB], start=(kc == 0), stop=(kc == 7))
                    for kc in range(8):
                        k.mm(pu[:, 0:TB], u_[:, kc, fc * 128:(fc + 1) * 128], h2[:, kc, tb:tb + TB], start=(kc == 0), stop=(kc == 7))
                    s_ = sa[fc % 2]
                    k.actv(s_.v, pa[:, 0:TB], AF.Silu)
                    k.tt(dve, hd_[:, fc, :], s_.v, pu[:, 0:TB], ALU.mult)

            def dn(tb):
                hd_ = hids[(tb // TB) % 2]
                for t3 in range(TB // 128):
                    tt = tb // 128 + t3
                    for nb in range(2):
                        py = k.ps()
                        for fc in range(4):
                            k.mm(py.v, hd_[:, fc, t3 * 128:(t3 + 1) * 128], d_[:, fc, nb * 512:(nb + 1) * 512], start=(fc == 0), stop=(fc == 3))
                        ya = y_acc[:, tt, nb * 512:(nb + 1) * 512]
                        if e == 0:
                            k.ts(dve, ya, py.v, gates[:, tt, e:e + 1], ALU.mult)
                        else:
                            k.stt(dve, ya, py.v, gates[:, tt, e:e + 1], ya, ALU.mult, ALU.add)

            tbs = list(range(0, NTH, TB))
            gu(tbs[0])
            for i_ in range(1, len(tbs)):
                gu(tbs[i_])
                dn(tbs[i_ - 1])
            dn(tbs[-1])
        _scw.__exit__(None, None, None)
        return y_acc

    def moe_final(hh, y_acc, stack):
        NTL = NTOK // 128
        NB = 3
        xts = [k.sb("fxt%d" % i, [128, D], F32, stack=stack) for i in range(NB)]
        tmps = [k.sb("ftmp%d" % i, [128, D], F32, stack=stack) for i in range(NB)]
        yo = [k.sb("fyo%d" % i, [128, D], F32, stack=stack) for i in range(NB)]
        tl = [(k.sb("fbn%d" % i, [128, 2, 6], F32, stack=stack), k.sb("fmv%d" % i, [128, 2], F32, stack=stack),
               k.sb("frs%d" % i, [128, 1], F32, stack=stack)) for i in range(NB)]
        gb, lb = load_bc(stack, 1, [0, 1], ("ln2_g", "ln2_b"))

        def stage_a(tt):
            cond = 0 if tt < 4 else 1
            xt, tmp, (stt_, mv, rs) = xts[tt % NB], tmps[tt % NB], tl[tt % NB]
            k.dma(xt.v, x1_t[tt * 128:(tt + 1) * 128, :])
            k.tt(dve, tmp.v, y_acc[:, tt, :], gb[cond].v, ALU.mult)
            k.stt(dve, tmp.v, xt.v, ALPHA, tmp.v, ALU.mult, ALU.add)
            for h_ in range(2):
                k.emit(dve, lambda h_=h_: nc.vector.bn_stats(out=stt_[:, h_, :].ap, in_=tmp[:, h_ * 512:(h_ + 1) * 512].ap), [tmp.v], [stt_.v])
            k.emit(dve, lambda: nc.vector.bn_aggr(out=mv.v.ap, in_=stt_.v.re("p a b -> p (a b)").ap), [stt_.v], [mv.v])
            k.ts(dve, rs.v, mv[:, 1:2], LN_EPS, ALU.add)
            k.actv(rs.v, rs.v, AF.Sqrt)
            k.recip(rs.v, rs.v)
            k.ts(dve, xt.v, tmp.v, mv[:, 0:1], ALU.subtract, rs.v, ALU.mult)

        def stage_b(tt):
            xt, yo_ = xts[tt % NB], yo[tt % NB]
            k.tt(dve, xt.v, xt.v, lb["ln2_g"].v, ALU.mult)
            k.tt(pool, yo_.v, xt.v, lb["ln2_b"].v, ALU.add)
            if tt < 4:
                k.dma(y_p[tt * 128:(tt + 1) * 128, :], yo_.v, is_output=True)
            else:
                k.dma(y_s[(tt - 4) * 128:(tt - 3) * 128, :], yo_.v, is_output=True)

        for tt in range(NTL + 1):
            if tt < NTL:
                stage_a(tt)
            if tt >= 1:
                stage_b(tt - 1)

    def s5_pass2(x_rows, L, nseq, cond, tok0, pos_rows, use_h0, st_out):
        NT = nseq * L
        J = L // 8
        with k.scope() as st:
            u_bf = k.sb("u_bf", [128, 4, NT], BF16, stack=st)
            with k.scope() as st2:
                h = [k.sb("h_fm%d" % i, [128, 8, 512], F32R, stack=st2) for i in range(NT // 512)]
                with k.scope() as st3:
                    ln_front(x_rows, NT, cond, h, st3, add_pos=pos_rows)
                proj_front(h, L, nseq, st2, u_bf, None, None, (3, 0, 1, 2), tok0=tok0)
            y_fm = k.sb("y_fm", [128, 4, NT], F32, stack=st)
            with k.scope() as stS:
                XsF = k.sb("Xs", [128, 2, G, nseq, J + 1], F32, stack=stS)
                XsB = k.alias(XsF, "XsB", stS)
                Xs = (XsF, XsB)
                U8 = k.sb("U8", [128, G, nseq * J], BF16, stack=stS)
                if use_h0:
                    k.cp(pool, XsF[F_, :, :, 0, 0], h0D[F_])
                    k.cp(dve, XsB[B_, :, :, 0, J], h0D[B_])
                else:
                    k.memset(pool, XsF[F_, :, :, :, 0], 0.0)
                    k.memset(dve, XsB[B_, :, :, :, J], 0.0)
                s5_front(u_bf, L, nseq, Xs, U8)
                if stage == 30 and st_out:
                    pass
                k.mark("scan%d" % L)
                with k.scope() as st2:
                    (s5_scan if J >= 64 else s5_scan_seq)(Xs, J, nseq, st2)
                k.mark("s5_back%d" % L)
                pass
                if st_out:
                    with k.scope() as st2:
                        fin = k.sb("fin", [128, 2, 2, G], F32, stack=st2)
                        for s in range(2):
                            k.cp(pool, fin[F_, s], XsF[F_, :, :, s, J])
                            k.cp(dve, fin[B_, s], XsB[B_, :, :, s, 0])
                        pf = k.ps()
                        for s in range(2):
                            for ri in range(2):
                                i = s * 2 + ri
                                k.tr(pf[0:32, i * 128:(i + 1) * 128], fin[:, s, ri, :], ident, inc=(i == 3))
                        stT = k.sb("stT", [32, 4, 128], F32, stack=st2)
                        k.cp(act, stT.v.re("g a q -> g (a q)"), pf[0:32, :])
                        for s in range(2):
                            k.dma(st_re_d[s].rearrange("d g p -> g d p"), stT[:, s * 2 + 0, :].re("g (d p) -> g d p", d=2), is_output=True)
                            k.dma(st_im_d[s].rearrange("d g p -> g d p"), stT[:, s * 2 + 1, :].re("g (d p) -> g d p", d=2), is_output=True)
                with k.scope() as st2:
                    s5_back(Xs, U8, u_bf, L, nseq, st2, y_fm)
            with k.scope() as st2:
                glu_rms(y_fm, NT, st2, mixS[:, :, tok0:tok0 + NT], 0, gcol=20)

    k.mark("s5_prompts")
    s5_pass2(xp_d, LP, 2, 0, 0, None, False, True)
    if stage in (30, 31):
        return k
    k.mark("s5_sample")
    s5_pass2(xs_d, LS, 1, 1, 2 * LP, pos_d, True, False)
    _scA.__exit__(None, None, None)

    def hy_pass(x_rows, L, nseq, cond, tok0, pos_rows):
        NT = nseq * L
        nf = L // 128
        with k.scope() as st:
            mixH = k.sb("mixH", [128, 4, NT], F32R, stack=st)
            x2 = k.sb("x2", [128, 4, NT], BF16, stack=st)
            vtm = k.sb("vtm", [128, nseq, nf, 512], BF16, stack=st)
            x1tm = k.sb("x1tm", [128, nseq, nf, 512], BF16, stack=st)
            with k.scope() as stV:
                vx1 = k.sb("vx1", [128, 8, NT], BF16, stack=stV)
                for ch in range(12):
                    dst_ = vx1[:, ch, :] if ch < 8 else x2[:, ch - 8, :]
                    k.dma(dst_, vx_t[ch, :, tok0:tok0 + NT])
                for s in range(nseq):
                    to_tm(vtm[:, s], vx1[:, 0:4, :], L, s)
                    to_tm(x1tm[:, s], vx1[:, 4:8, :], L, s)
            with k.scope() as stK:
                Kt = k.sb("Kt", [128, nf, 2, 2, 512], BF16, stack=stK)
                k.dma(Kt.v.re("p a b c d -> p (a b c d)"), kt_t[L].v)
                k.mark("hy_conv%d" % L)
                DF = k.sb("DF", [128, 2, nf, L], BF16, stack=stK)
                k.dma(DF.v, dft_d[L][:, 0:2])
                hyena_conv(L, nseq, Kt, DF, vtm, x1tm, x2, stK, mixH)
            with k.scope() as st2:
                rms_mix(mixH.v.bitcast(F32), NT, st2, mixH.v, 0, gcol=16)
            k.mark("out_proj%d" % L)
            with k.scope() as st2:
                out_proj_ln1(mixH, mixS[:, :, tok0:tok0 + NT], x_rows, NT, cond, st2, tok0, add_pos=pos_rows)

    k.mark("hy_prompts")
    hy_pass(xp_d, LP, 2, 0, 0, None)
    k.mark("hy_sample")
    hy_pass(xs_d, LS, 1, 1, 2 * LP, pos_d)
    _scB.__exit__(None, None, None)
    k.mark("moe")

    with k.scope() as st:
        ya = moe_half(0, st)
        with k.scope() as st2:
            moe_final(0, ya, st2)
    k.finish()

    return k


def _prep_inputs(inp):
    c = _host_consts()
    f = lambda a: np.ascontiguousarray(np.asarray(a, np.float32))
    shared = {
        "pos": c["pos"], "cst": c["cst"], "cstb": c["cstb"],
        "w_ada": f(inp["w_ada"][0]), "w_in": f(inp["w_in"][0]),
        "ln1_g": f(inp["ln1_g"]), "ln1_b": f(inp["ln1_b"]), "ln2_g": f(inp["ln2_g"]), "ln2_b": f(inp["ln2_b"]),
        "s5_a_re": f(inp["s5_a_re"][0]), "s5_a_im": f(inp["s5_a_im"][0]), "s5_log_dt": f(inp["s5_log_dt"][0]),
        "s5_b_re": f(inp["s5_b_re"][0]), "s5_b_im": f(inp["s5_b_im"][0]),
        "s5_c_re": f(inp["s5_c_re"][0]), "s5_c_im": f(inp["s5_c_im"][0]),
        "s5_w_glu": f(inp["s5_w_glu"][0]),
        "w_out": f(inp["w_out"][0]),
        "moe_wr": np.ascontiguousarray(np.concatenate([f(inp["moe_w_r1"][0]), f(inp["moe_w_r2"][0]).transpose(1, 0, 2).reshape(D, 16)], axis=1)),
        "moe_br": np.ascontiguousarray(np.concatenate([f(inp["moe_b_r1"][0]), f(inp["moe_b_r2"][0]).reshape(16)])[None, :]),
        "moe_w_gate": f(inp["moe_w_gate"][0]), "moe_w_up": f(inp["moe_w_up"][0]), "moe_w_down": f(inp["moe_w_down"][0]),
        "hy_f_w1": f(inp["hy_f_w1"][0]), "hy_f_w2": f(inp["hy_f_w2"][0]), "hy_f_w3": f(inp["hy_f_w3"][0]),
        "hy_f_b1": f(inp["hy_f_b1"][0]).reshape(64, 1), "hy_f_b2": f(inp["hy_f_b2"][0]).reshape(64, 1),
        "hy_freq": f(inp["hy_freq"][0]).reshape(64, 1), "hy_fbias": f(inp["hy_fbias"][0]).reshape(1, 1024),
        "zT1024": c["zT1024"], "zT256": c["zT256"], "dec1024": c["dec1024"], "dec256": c["dec256"],
        "dft1024": c["dft1024"], "dft256": c["dft256"],
    }
    maps = []
    for i in range(NCORE):
        m = dict(shared)
        m["xs"] = f(inp["x_sample"][i])
        m["xp"] = f(inp["x_prompt"][2 * i:2 * i + 2]).reshape(2 * LP, D)
        cc = np.stack([f(inp["c_ctx"]), f(inp["c"][i])], 0)
        vec = np.concatenate([cc.reshape(-1), f(inp["out_norm_g"][0]), f(inp["hy_conv_w"][0]).reshape(-1),
                              f(inp["hy_conv_b"][0]), f(inp["s5_d"][0]), f(inp["s5_b_glu"][0]), f(inp["b_ada"][0])])
        m["vecs"] = np.ascontiguousarray(vec.reshape(128, 128))
        m["h0"] = np.ascontiguousarray(np.stack([f(inp["state_s5_re"][i, 0]), f(inp["state_s5_im"][i, 0])], 0))
        maps.append(m)
    return maps


_CACHE = {}


def kernel(**inp):
    if "k" not in _CACHE:
        _CACHE["k"] = build()
    k = _CACHE["k"]
    maps = _prep_inputs(inp)
    res = run_bass_kernel_spmd(k.nc, maps, core_ids=list(range(NCORE)))
    r = res.results
    y_prompt = np.concatenate([r[i]["y_p"].reshape(2, LP, D) for i in range(NCORE)], 0).astype(np.float32)
    y_sample = np.stack([r[i]["y_s"] for i in range(NCORE)], 0).astype(np.float32)
    st_re = np.concatenate([r[i]["st_re"] for i in range(NCORE)], 0).reshape(2 * NCORE, 1, 2, G, P).astype(np.float32)
    st_im = np.concatenate([r[i]["st_im"] for i in range(NCORE)], 0).reshape(2 * NCORE, 1, 2, G, P).astype(np.float32)
    return (y_prompt, y_sample, st_re, st_im)
```

```python
import math
import contextlib
import numpy as np
import ml_dtypes
import concourse.bass as bass
import concourse.mybir as mybir
from concourse.bass_utils import run_bass_kernel_spmd

F32 = mybir.dt.float32
BF16 = mybir.dt.bfloat16
F32R = mybir.dt.float32r
AF = mybir.ActivationFunctionType
ALU = mybir.AluOpType
AX = mybir.AxisListType

D = 1024
NCORE = 8
LS, LP = 1024, 256
NTOK = LS + 2 * LP
G, P, H = 32, 64, 16
T8 = 8
LN_EPS = 1e-5
ALPHA = 2.0 ** 0.25
MAGIC = 12582912.0
TWO_PI = 2.0 * math.pi


class Eng:
    def __init__(self, name, h):
        self.name, self.h = name, h
        self.sem = None
        self.cnt = 0
        self.seen = {}


class T:
    def __init__(self, ap, name):
        self.ap, self.name = ap, name
        self.w = None
        self.r = {}

    def __getitem__(self, key):
        return V(self, self.ap[key])

    @property
    def v(self):
        return V(self, self.ap)


class V:
    def __init__(self, t, ap):
        self.t, self.ap = t, ap

    def __getitem__(self, key):
        return V(self.t, self.ap[key])

    def bitcast(self, d):
        return V(self.t, self.ap.bitcast(d))

    def bc(self, shape):
        return V(self.t, self.ap.broadcast_to(shape))

    def re(self, pat, **kw):
        return V(self.t, self.ap.rearrange(pat, **kw))


def _ap(x):
    return x.ap if isinstance(x, V) else x


class KB:
    def __init__(self):
        nc = bass.Bass("TRN2", target_bir_lowering=False)
        nc.dge_precook = False
        self.nc = nc
        self.es = contextlib.ExitStack()
        self.pe = Eng("pe", nc.tensor)
        self.act = Eng("act", nc.scalar)
        self.dve = Eng("dve", nc.vector)
        self.pool = Eng("pool", nc.gpsimd)
        self.sp = Eng("sp", nc.sync)
        for e in (self.pe, self.act, self.dve, self.pool, self.sp):
            e.sem = self.es.enter_context(nc.semaphore("s_" + e.name))
        self.ndsem = 32
        self.dsem = [self.es.enter_context(nc.semaphore("s_dma%d" % i)) for i in range(self.ndsem)]
        self.dval = [0] * self.ndsem
        self.dnext = 0
        self.dnext_sw = 0
        self.out_waits = []
        self.nalloc = 0
        self.freed = []
        self.stq = self.pool
        self.npe = 0
        self.marks = []
        self.psum = [self.sb("psb%d" % i, [128, 512], F32, psum=True) for i in range(8)]
        self.psn = 0

    @contextlib.contextmanager
    def scope(self):
        st = contextlib.ExitStack()
        st._tiles = []
        try:
            yield st
        finally:
            for t in st._tiles:
                deps = {}
                for dep in ([t.w] if t.w else []) + list(t.r.values()):
                    kind, key, val = dep
                    skey = key.name if kind == "e" else "d%d" % key
                    if skey not in deps or deps[skey][2] < val:
                        deps[skey] = dep
                if deps:
                    self.freed.append((t.lo, t.hi, deps))
            st.close()

    def sb(self, name, shape, dtype, psum=False, stack=None):
        self.nalloc += 1
        nm = "%s_%d" % (name, self.nalloc)
        cm = (self.nc.psum_tensor if psum else self.nc.sbuf_tensor)(nm, shape, dtype)
        t = (stack or self.es).enter_context(cm)
        tt_ = T(t[:], nm)
        if not psum:
            ml = self.nc.lookup_mloc(nm)
            tt_.lo = int(ml.addr)
            tt_.hi = tt_.lo + int(ml.dims[1])
            keep = []
            for lo, hi, deps in self.freed:
                if lo < tt_.hi and tt_.lo < hi:
                    for skey, dep in deps.items():
                        if skey not in tt_.r or tt_.r[skey][2] < dep[2]:
                            tt_.r[skey] = dep
                    if not (tt_.lo <= lo and hi <= tt_.hi):
                        keep.append((lo, hi, deps))
                else:
                    keep.append((lo, hi, deps))
            self.freed = keep
            if stack is not None and hasattr(stack, "_tiles"):
                stack._tiles.append(tt_)
        return tt_

    def alias(self, t, name, stack):
        t2 = T(t.ap, name)
        t2.lo, t2.hi = t.lo, t.hi
        t2.r = dict(t.r)
        if stack is not None and hasattr(stack, "_tiles"):
            stack._tiles.append(t2)
        return t2

    def ps(self):
        t = self.psum[self.psn % 8]
        self.psn += 1
        return t

    def _need(self, eng, dep, waits):
        if dep is None:
            return
        kind, key, val = dep
        if kind == "e":
            if key is eng and eng is self.pe:
                return
            skey = key.name
        else:
            skey = "d%d" % key
        if eng.seen.get(skey, 0) >= val:
            return
        prev = waits.get(skey)
        if prev is None or prev[1] < val:
            waits[skey] = (key.sem if kind == "e" else self.dsem[key], val)

    def _deps(self, eng, reads, writes):
        waits = {}
        for v in reads:
            if isinstance(v, V):
                self._need(eng, v.t.w, waits)
        for v in writes:
            if isinstance(v, V):
                self._need(eng, v.t.w, waits)
                for dep in v.t.r.values():
                    self._need(eng, dep, waits)
        for skey, (sem, val) in waits.items():
            eng.h.wait_ge(sem, val)
            eng.seen[skey] = val

    def emit(self, eng, fn, reads, writes, inc=True):
        self._deps(eng, reads, writes)
        inst = fn()
        if inc:
            inst.then_inc(eng.sem, 1)
            eng.cnt += 1
            c = eng.cnt
            eng.pending = False
        else:
            c = eng.cnt + 1
            eng.pending = True
        for v in reads:
            if isinstance(v, V):
                v.t.r[eng.name] = ("e", eng, c)
        for v in writes:
            if isinstance(v, V):
                v.t.w = ("e", eng, c)
                v.t.r = {}
        return inst

    def dma(self, out, in_, q=None, is_output=False):
        is_store = isinstance(in_, V) and getattr(in_.t, "lo", None) is not None
        eng = q or (self.stq if is_store else self.sp)
        if eng is self.pool:
            i = 20 + self.dnext_sw % 12
            self.dnext_sw += 1
        else:
            i = self.dnext % 20
            self.dnext += 1
        reads = [in_] if isinstance(in_, V) else []
        writes = [out] if isinstance(out, V) else []
        self._deps(eng, reads, writes)
        if self.dval[i] > 0 and eng.seen.get("d%d" % i, 0) < self.dval[i]:
            eng.h.wait_ge(self.dsem[i], self.dval[i])
            eng.seen["d%d" % i] = self.dval[i]
        self.dval[i] += 16
        eng.h.dma_start(out=_ap(out), in_=_ap(in_)).then_inc(self.dsem[i], 16)
        dep = ("d", i, self.dval[i])
        for v in reads:
            v.t.r["d%d" % i] = dep
        for v in writes:
            v.t.w = dep
            v.t.r = {}
        if is_output:
            self.out_waits.append((i, self.dval[i]))

    def finish(self):
        for i, val in self.out_waits:
            if self.sp.seen.get("d%d" % i, 0) < val:
                self.sp.h.wait_ge(self.dsem[i], val)
                self.sp.seen["d%d" % i] = val
        for e in (self.pe, self.act, self.dve, self.pool):
            if e.cnt:
                self.sp.h.wait_ge(e.sem, e.cnt)

    def mark(self, name):
        self.marks.append((name, self.npe))

    def mm(self, out, lhsT, rhs, start=True, stop=True, inc=None):
        self.npe += 1
        if inc is None:
            inc = stop
        return self.emit(self.pe, lambda: self.nc.tensor.matmul(_ap(out), _ap(lhsT), _ap(rhs), start=start, stop=stop),
                         [lhsT, rhs], [out], inc=inc)

    def tr(self, out, in_, ident, inc=True):
        self.npe += 1
        return self.emit(self.pe, lambda: self.nc.tensor.transpose(_ap(out), _ap(in_), _ap(ident)),
                         [in_, ident], [out], inc=inc)

    def actv(self, out, in_, func, scale=1.0, bias=None, accum_out=None):
        reads = [in_] + [x for x in (scale, bias) if isinstance(x, V)]
        writes = [out] + ([accum_out] if accum_out is not None else [])
        kw = {}
        if bias is not None:
            kw["bias"] = _ap(bias)
        if accum_out is not None:
            kw["accum_out"] = _ap(accum_out)
        return self.emit(self.act, lambda: self.nc.scalar.activation(out=_ap(out), in_=_ap(in_), func=func,
                                                                      scale=_ap(scale), **kw), reads, writes)

    def tt(self, eng, out, in0, in1, op):
        return self.emit(eng, lambda: eng.h.tensor_tensor(out=_ap(out), in0=_ap(in0), in1=_ap(in1), op=op),
                         [in0, in1], [out])

    def ts(self, eng, out, in0, s1, op0, s2=None, op1=None):
        reads = [in0] + [x for x in (s1, s2) if isinstance(x, V)]
        if op1 is None:
            return self.emit(eng, lambda: eng.h.tensor_scalar(out=_ap(out), in0=_ap(in0), scalar1=_ap(s1),
                                                              scalar2=None, op0=op0), reads, [out])
        return self.emit(eng, lambda: eng.h.tensor_scalar(out=_ap(out), in0=_ap(in0), scalar1=_ap(s1),
                                                          scalar2=_ap(s2), op0=op0, op1=op1), reads, [out])

    def stt(self, eng, out, in0, scalar, in1, op0, op1):
        reads = [in0, in1] + ([scalar] if isinstance(scalar, V) else [])
        return self.emit(eng, lambda: eng.h.scalar_tensor_tensor(out=_ap(out), in0=_ap(in0), scalar=_ap(scalar),
                                                                 in1=_ap(in1), op0=op0, op1=op1), reads, [out])

    def cp(self, eng, out, in_):
        if eng is self.act:
            return self.emit(eng, lambda: self.nc.scalar.copy(out=_ap(out), in_=_ap(in_)), [in_], [out])
        return self.emit(eng, lambda: eng.h.tensor_copy(out=_ap(out), in_=_ap(in_)), [in_], [out])

    def memset(self, eng, out, val):
        return self.emit(eng, lambda: eng.h.memset(_ap(out), val), [], [out])

    def recip(self, out, in_):
        return self.emit(self.dve, lambda: self.nc.vector.reciprocal(out=_ap(out), in_=_ap(in_)), [in_], [out])

    def reduce(self, out, in_, op, axis=None):
        return self.emit(self.dve, lambda: self.nc.vector.tensor_reduce(out=_ap(out), in_=_ap(in_), axis=axis or AX.X, op=op),
                         [in_], [out])


def _bf(x):
    return np.ascontiguousarray(x.astype(ml_dtypes.bfloat16))


def _host_consts():
    c = {}
    ident = np.eye(128, dtype=np.float32)
    rr = np.arange(128) // 16
    maskF = (rr[None, :] >= rr[:, None]).astype(np.float32)
    maskB = (rr[:, None] >= rr[None, :]).astype(np.float32)
    selc = np.zeros((128, 256), np.float32)
    selc[0, 0:128] = 1.0
    selc[1, 128:256] = 1.0
    ones = np.ones((128, 128), np.float32)
    c["cst"] = np.ascontiguousarray(np.concatenate([ident, maskF, maskB, selc, ones], axis=1))
    sel = np.zeros((128, 64, 128), np.float32)
    for g8 in range(8):
        for r in range(8):
            for h in range(16):
                sel[g8 * 16 + h, g8 * 8 + r, r * 16 + h] = 1.0
    c["cstb"] = _bf(np.concatenate([ident, ones, sel.reshape(128, 64 * 128)], axis=1))
    for L in (LS, LP):
        s = np.arange(L, dtype=np.float64)[:, None]
        f = np.arange(L, dtype=np.float64)[None, :]
        ang2 = np.pi * (2 * f + 1) * (2 * s + 1) / (4 * L)
        angk = np.pi * (2 * f + 1) * s / (2 * L)
        mats = np.stack([np.cos(ang2), np.sin(ang2), np.cos(angk), np.sin(angk)], 0)
        c["dft%d" % L] = _bf(mats.reshape(4, L // 128, 128, L).transpose(2, 0, 1, 3))
        t = np.linspace(0.0, 1.0, L, dtype=np.float32)[:, None]
        w = (2.0 * np.pi * np.arange(L, dtype=np.float32) / L).astype(np.float32)
        fb = np.linspace(1e-4, 15, 16, dtype=np.float32)
        ang = w[:, None] * fb[None, :]
        z = np.concatenate([t, np.cos(ang), -np.sin(ang)], axis=-1).astype(np.float32)
        c["zT%d" % L] = np.ascontiguousarray(z.T)
        deltas = np.linspace(math.log(1e-2) / 1.5, math.log(1e-2) / 0.3, 512, dtype=np.float32)
        dec = np.exp(-t * np.abs(deltas)[None, :]).astype(np.float32)
        c["dec%d" % L] = np.ascontiguousarray(dec.reshape(L // 128, 128, 512).transpose(1, 0, 2))
    rows = LS // 64
    row = np.repeat(np.arange(rows, dtype=np.float32), 64)
    col = np.tile(np.arange(64, dtype=np.float32), rows)
    q = D // 4
    omega = (1.0 / (10000.0 ** (np.arange(q, dtype=np.float32) / q))).astype(np.float32)
    er = row[:, None] * omega
    ec = col[:, None] * omega
    c["pos"] = np.concatenate([np.sin(er), np.cos(er), np.sin(ec), np.cos(ec)], axis=-1).astype(np.float32)
    return c


def build(stage=99):
    k = KB()
    nc = k.nc
    pe, act, dve, pool, sp = k.pe, k.act, k.dve, k.pool, k.sp
    dbg = {}

    def din(name, shape, dtype=F32):
        return nc.dram_tensor(name, list(shape), dtype, kind="ExternalInput").ap()

    def dout(name, shape, dtype=F32):
        return nc.dram_tensor(name, list(shape), dtype, kind="ExternalOutput").ap()

    xs_d = din("xs", [LS, D])
    xp_d = din("xp", [2 * LP, D])
    pos_d = din("pos", [LS, D])
    vecs_d = din("vecs", [128, 128])
    h0_d = din("h0", [2, 2, G, P])
    cst_d = din("cst", [128, 768])
    cstb_d = din("cstb", [128, 256 + 8192], BF16)
    w_ada_d = din("w_ada", [D, 6 * D], F32R)
    w_in_d = din("w_in", [D, 2048], F32R)
    w_glu_d = din("s5_w_glu", [512, 512], F32R)
    w_out_d = din("w_out", [D, D], F32R)
    wr_d = din("moe_wr", [D, 20], F32R)
    br_d = din("moe_br", [1, 20])
    wgate_d = din("moe_w_gate", [16, D, 512], F32R)
    wup_d = din("moe_w_up", [16, D, 512], F32R)
    wdown_d = din("moe_w_down", [16, 512, D], F32R)
    x1_t = T(nc.dram_tensor("x1_scratch", [NTOK, D], F32, kind="Internal").ap(), "x1_scratch")
    h2_t = T(nc.dram_tensor("h2_scratch", [128, 8, NTOK], F32R, kind="Internal").ap(), "h2_scratch")
    kt_t = {L: T(nc.dram_tensor("kt_scratch%d" % L, [128, (L // 128) * 2 * 2 * 512], BF16, kind="Internal").ap(), "kt_scratch%d" % L)
            for L in (LS, LP)}
    vx_t = T(nc.dram_tensor("vx_scratch", [12, 128, NTOK], BF16, kind="Internal").ap(), "vx_scratch")
    hyw1_d = din("hy_f_w1", [33, 64])
    hyw2_d = din("hy_f_w2", [64, 64])
    hyw3_d = din("hy_f_w3", [64, 2048])
    hyb1_d = din("hy_f_b1", [64, 1])
    hyb2_d = din("hy_f_b2", [64, 1])
    hyfr_d = din("hy_freq", [64, 1])
    hyfb_d = din("hy_fbias", [1, 1024])
    zT_d = {L: din("zT%d" % L, [33, L]) for L in (LS, LP)}
    dec_d = {L: din("dec%d" % L, [128, L // 128, 512]) for L in (LS, LP)}
    dft_d = {L: din("dft%d" % L, [128, 4, L // 128, L], BF16) for L in (LS, LP)}
    y_s = dout("y_s", [LS, D])
    y_p = dout("y_p", [2 * LP, D])

    cst = k.sb("cst", [128, 768], F32)
    k.dma(cst.v, cst_d)
    ident = cst[:, 0:128]
    maskF = cst[:, 128:256]
    maskB = cst[:, 256:384]
    selc = cst[0:2, 384:640]
    ones = cst[:, 640:768]
    cstb = k.sb("cstb", [128, 256], BF16)
    k.dma(cstb.v, cstb_d[:, 0:256])
    ident_b = cstb[:, 0:128]
    ones_b = cstb[:, 128:256]
    selh = {}

    def sel(i):
        return selh["t"][:, i * 128:(i + 1) * 128]

    vecT = k.sb("vecT", [128, 128], F32)
    with k.scope() as stv:
        vrow = k.sb("vrow", [128, 128], F32, stack=stv)
        k.dma(vrow.v, vecs_d)
        pt = k.ps()
        k.tr(pt[:, 0:128], vrow.v, ident)
        k.cp(dve, vecT.v, pt[:, 0:128])
    condS = k.sb("condS", [128, 8, 2], F32R)
    k.actv(condS.v.re("p kc n -> p n kc"), vecT[:, 0:16].re("p (n kc) -> p n kc", n=2), AF.Silu)

    def hyena_filter(L, Ktab):
        nf = L // 128
        with k.scope() as st:
            sbt = lambda n, s, d=F32: k.sb(n, s, d, stack=st)
            w3 = sbt("hw3", [64, 2048]); k.dma(w3.v, hyw3_d)
            fb = sbt("hfb", [128, 2, 512]); k.dma(fb.v.re("p a b -> p (a b)"), hyfb_d.broadcast_to([128, 1024]))
            k.ts(dve, fb.v, fb.v, 1.0 / L, ALU.mult)
            h2 = sbt("fh2", [64, L])
            with k.scope() as stm:
                sbm = lambda n, s, d=F32: k.sb(n, s, d, stack=stm)
                zT = sbm("zT", [33, L])
                k.dma(zT.v, zT_d[L])
                w1 = sbm("hw1", [33, 64]); k.dma(w1.v, hyw1_d)
                w2 = sbm("hw2", [64, 64]); k.dma(w2.v, hyw2_d)
                b1 = sbm("hb1", [64, 1]); k.dma(b1.v, hyb1_d)
                b2 = sbm("hb2", [64, 1]); k.dma(b2.v, hyb2_d)
                fr = sbm("hfr", [64, 1]); k.dma(fr.v, hyfr_d)
                h1 = sbm("fh1", [64, L])
                pre = sbm("fpre", [64, 512]); kk_ = sbm("fkk", [64, 512])

                def layer(hout, hin, W, b):
                    for t0 in range(0, L, 512):
                        n = min(512, L - t0)
                        pl = k.ps()
                        k.mm(pl[0:64, 0:n], W, hin[:, t0:t0 + n])
                        k.ts(dve, pre[:, 0:n], pl[0:64, 0:n], b.v, ALU.add, fr.v, ALU.mult)
                        k.ts(dve, kk_[:, 0:n], pre[:, 0:n], 1.0 / TWO_PI, ALU.mult, MAGIC, ALU.add)
                        k.ts(dve, kk_[:, 0:n], kk_[:, 0:n], MAGIC, ALU.subtract)
                        k.stt(dve, pre[:, 0:n], kk_[:, 0:n], -TWO_PI, pre[:, 0:n], ALU.mult, ALU.add)
                        k.ts(dve, pre[:, 0:n], pre[:, 0:n], 3.14159, ALU.min, -3.14159, ALU.max)
                        k.actv(hout[:, t0:t0 + n], pre[:, 0:n], AF.Sin)

                layer(h1, zT.v, w1.v, b1)
                layer(h2, h1.v, w2.v, b2)
            FK = k.sb("FK", [128, 2, nf, L], BF16, stack=st)
            k.dma(FK.v, dft_d[L][:, 2:4])
            dec = sbt("dec", [128, nf, 512]); k.dma(dec.v, dec_d[L])
            he = k.sb("he", [128, nf, 512], BF16, stack=st)
            ho = k.sb("ho", [128, nf, 512], BF16, stack=st)
            hds = [sbt("hd%d" % i, [128, 2, 512]) for i in range(2)]
            for o in range(2):
                for s_ in range(nf):
                    hd = hds[s_ % 2]
                    pq = [k.ps() for _ in range(2)]
                    for d_ in range(2):
                        q = 2 * o + d_
                        k.mm(pq[d_].v, h2[:, s_ * 128:(s_ + 1) * 128], w3[:, q * 512:(q + 1) * 512])
                    for d_ in range(2):
                        k.tt(dve, hd[:, d_, :], pq[d_].v, dec[:, s_, :], ALU.mult)
                    if s_ == 0:
                        k.memset(dve, hd[0:1, 1, :], 0.0)
                    k.tt(dve, he[:, s_, :], hd[:, 0, :], hd[:, 1, :], ALU.add)
                    k.tt(pool, ho[:, s_, :], hd[:, 0, :], hd[:, 1, :], ALU.subtract)
                for ft in range(nf):
                    pr = k.ps(); pi_ = k.ps()
                    for s_ in range(nf):
                        k.mm(pr.v, FK[:, 0, s_, ft * 128:(ft + 1) * 128], he[:, s_, :], start=(s_ == 0), stop=(s_ == nf - 1))
                    for s_ in range(nf):
                        k.mm(pi_.v, FK[:, 1, s_, ft * 128:(ft + 1) * 128], ho[:, s_, :], start=(s_ == 0), stop=(s_ == nf - 1))
                    k.stt(dve, Ktab[:, ft, 0, o, :], pr.v, 1.0 / L, fb[:, o, :], ALU.mult, ALU.add)
                    k.actv(Ktab[:, ft, 1, o, :], pi_.v, AF.Copy, scale=-1.0 / L)

    modT = k.sb("modT", [128, 48, 2], F32)
    _scm = k.scope()
    st_mod = _scm.__enter__()
    wa = [k.sb("wa%d" % i, [128, 8, 512], F32R, stack=st_mod) for i in range(2)]
    mstg = [k.sb("mstg%d" % i, [2, 512], F32, stack=st_mod) for i in range(2)]
    w_ada_v = w_ada_d.rearrange("(kc p) n -> p kc n", p=128)
    for nb in range(2):
        k.dma(wa[nb].v, w_ada_v[:, :, nb * 512:(nb + 1) * 512])
    k.mark("filters")
    for L_ in (LS, LP):
        with k.scope() as stf:
            Kt0 = k.sb("Kt0", [128, L_ // 128, 2, 2, 512], BF16, stack=stf)
            hyena_filter(L_, Kt0)
            k.dma(kt_t[L_].v, Kt0.v.re("p a b c d -> p (a b c d)"))
    k.mark("mod")

    for nb in range(12):
        wt = wa[nb % 2]
        if nb >= 2:
            k.dma(wt.v, w_ada_v[:, :, nb * 512:(nb + 1) * 512])
        pm = k.ps()
        for kc in range(8):
            k.mm(pm[0:2, :], condS[:, kc, :], wt[:, kc, :], start=(kc == 0), stop=(kc == 7))
        sg = mstg[nb % 2]
        k.cp(act, sg.v, pm[0:2, :])
        pT = k.ps()
        for q in range(4):
            k.tr(pT[:, 2 * q:2 * q + 2], sg[:, q * 128:(q + 1) * 128], ident[0:2, 0:2], inc=(q == 3))
        k.tt(dve, modT[:, 4 * nb:4 * nb + 4, :], pT[:, 0:8].re("p (c n) -> p c n", n=2),
             vecT[:, 80 + 4 * nb:84 + 4 * nb].re("p (c o) -> p c o", o=1).bc([128, 4, 2]), ALU.add)
    k.ts(dve, modT[:, 8:16, :], modT[:, 8:16, :], 1.0, ALU.add)
    k.ts(dve, modT[:, 32:40, :], modT[:, 32:40, :], 1.0, ALU.add)
    _scm.__exit__(None, None, None)
    ln_d = {nm: din(nm, [1, D]) for nm in ("ln1_g", "ln1_b", "ln2_g", "ln2_b")}

    def load_bc(stack, which, conds, lnames):
        gb = {}
        dg = k.sb("dg", [128, 128], F32, stack=stack)
        for cond in conds:
            t = k.sb("gbc%d" % cond, [128, D], F32, stack=stack)
            for hb in range(2):
                pb = k.ps()
                for q in range(4):
                    c = (16 if which == 0 else 40) + hb * 4 + q
                    k.ts(dve, dg.v, ident, modT[:, c, cond:cond + 1], ALU.mult)
                    k.mm(pb[:, q * 128:(q + 1) * 128], ones, dg.v)
                k.cp(act, t[:, hb * 512:(hb + 1) * 512], pb.v)
            gb[cond] = t
        lb = {}
        for nm in lnames:
            t = k.sb("bc_" + nm, [128, D], F32, stack=stack)
            k.dma(t.v, ln_d[nm].broadcast_to([128, D]))
            lb[nm] = t
        return gb, lb

    k.mark("s5prep")
    a_d = [din("s5_a_re", [2, G, P]), din("s5_a_im", [2, G, P])]
    ldt_d = din("s5_log_dt", [2, G])
    b_d = [din("s5_b_re", [2, G, P, H]), din("s5_b_im", [2, G, P, H])]
    c_d = [din("s5_c_re", [G, H, P]), din("s5_c_im", [G, H, P])]
    epsc = k.sb("epsc", [128, 1], F32)
    k.memset(dve, epsc.v, LN_EPS)
    _scB = k.scope()
    stB = _scB.__enter__()
    mixS = k.sb("mixS", [128, 4, NTOK], F32R, stack=stB)
    _scA = k.scope()
    stA = _scA.__enter__()
    selh["t"] = k.sb("selt", [128, 8192], BF16, stack=stA)
    k.dma(selh["t"].v, cstb_d[:, 256:256 + 8192])
    WT = k.sb("WT", [128, G, 2, 128], BF16, stack=stA)
    V2 = k.sb("V2", [128, G, 2, 128], BF16, stack=stA)
    Kmat = k.sb("Kmat", [128, G, 128], BF16, stack=stA)
    mu = k.sb("mu", [128, 2, G], F32, stack=stA)
    PT = k.sb("PT", [128, 2, G, 16], F32, stack=stA)
    mu16 = k.sb("mu16", [128, 3, G], F32, stack=stA)
    F_q, B_q = slice(0, 64), slice(64, 128)
    h0D = k.sb("h0D", [128, 2, G], F32, stack=stA)
    with k.scope() as st:
        sbt = lambda n, s, d=F32: k.sb(n, s, d, stack=st)
        nat = sbt("nat", [32, 4, 128])
        for ri in range(2):
            k.dma(nat[:, ri, :].re("g (d p) -> g d p", d=2), a_d[ri].rearrange("d g p -> g d p"))
            k.dma(nat[:, 2 + ri, :].re("g (d p) -> g d p", d=2), h0_d[ri].rearrange("d g p -> g d p"))
        pa = k.ps()
        for i in range(4):
            k.tr(pa[:, i * 32:(i + 1) * 32], nat[:, i, :], ident[0:32, 0:32], inc=(i == 3))
        aD = sbt("aD", [128, 2, G])
        k.cp(dve, aD.v.re("p a g -> p (a g)"), pa[:, 0:64])
        k.cp(dve, h0D.v.re("p a g -> p (a g)"), pa[:, 64:128])
        ldt = sbt("ldt", [128, G])
        for d_ in range(2):
            k.dma(ldt[d_ * 64:(d_ + 1) * 64, :], ldt_d[d_:d_ + 1, :].broadcast_to([64, G]))
        bD = sbt("bD", [128, 2, G, H])
        for d_ in range(2):
            for ri in range(2):
                k.dma(bD[d_ * 64:(d_ + 1) * 64, ri], b_d[ri][d_].rearrange("g p h -> p g h"))
        cnat = sbt("cnat", [128, 4, 2, 128])
        for ri in range(2):
            for dup in range(2):
                k.dma(cnat[:, :, ri, dup * 64:(dup + 1) * 64], c_d[ri].rearrange("(cc g8) h p -> (g8 h) cc p", cc=4))
        cD = sbt("cD", [128, 2, G, H])
        for ri in range(2):
            pc = k.ps()
            for cc in range(4):
                k.tr(pc[:, cc * 128:(cc + 1) * 128], cnat[:, cc, ri, :], ident, inc=(cc == 3))
            k.cp(act, cD[:, ri].re("p g h -> p (g h)"), pc.v)

        if stage == 13:
            dd = dout("dbg_aD", [128, 2 * G])
            k.dma(dd, aD.v.re("p a g -> p (a g)"), is_output=True)
            dd2 = dout("dbg_cD", [128, 2 * G * H])
            k.dma(dd2, cD.v.re("p a g h -> p (a g h)"), is_output=True)
            dd3 = dout("dbg_bD", [128, 2 * G * H])
            k.dma(dd3, bD.v.re("p a g h -> p (a g h)"), is_output=True)
            dd4 = dout("dbg_ldt", [128, G])
            k.dma(dd4, ldt.v, is_output=True)
            k.finish()
            return k
        tmp = [sbt("ptmp%d" % i, [128, G]) for i in range(8)]

        def horner(out, y, coefs):
            n = len(coefs) - 1
            k.ts(dve, out, y, float(coefs[n]), ALU.mult)
            for kk in range(n - 1, 0, -1):
                k.stt(dve, out, out, float(coefs[kk]), y, ALU.add, ALU.mult)
            k.ts(dve, out, out, float(coefs[0]), ALU.add)

        fact = [1.0 / math.factorial(i) for i in range(12)]

        def exp_acc(out, x, nsq, scratch):
            k.ts(dve, scratch, x, 1.0 / (2 ** nsq), ALU.mult)
            horner(out, scratch, fact[0:12])
            for _ in range(nsq):
                k.tt(dve, out, out, out, ALU.mult)

        dtt = sbt("dtt", [128, G])
        exp_acc(dtt.v, ldt.v, 3, tmp[0].v)
        er = sbt("er", [128, G])
        th = sbt("th", [128, G])
        k.tt(dve, er.v, aD[:, 0, :], dtt.v, ALU.mult)
        k.tt(dve, th.v, aD[:, 1, :], dtt.v, ALU.mult)
        mag = sbt("mag", [128, G])
        exp_acc(mag.v, er.v, 2, tmp[0].v)
        kk_ = tmp[1]
        k.ts(dve, kk_.v, th.v, 1.0 / TWO_PI, ALU.mult, MAGIC, ALU.add)
        k.ts(dve, kk_.v, kk_.v, MAGIC, ALU.subtract)
        C1 = 6.28125
        C2 = TWO_PI - C1
        rq = tmp[2]
        k.stt(dve, rq.v, kk_.v, -C1, th.v, ALU.mult, ALU.add)
        k.stt(dve, rq.v, kk_.v, -C2, rq.v, ALU.mult, ALU.add)
        k.ts(dve, rq.v, rq.v, 0.25, ALU.mult)
        z2 = tmp[3]
        k.tt(dve, z2.v, rq.v, rq.v, ALU.mult)
        sn = sbt("sn", [128, G])
        cs = sbt("cs", [128, G])
        horner(sn.v, z2.v, [1.0, -fact[3], fact[5], -fact[7], fact[9], -fact[11]])
        k.tt(dve, sn.v, sn.v, rq.v, ALU.mult)
        horner(cs.v, z2.v, [1.0, -fact[2], fact[4], -fact[6], fact[8], -fact[10]])
        for _ in range(2):
            k.tt(dve, tmp[4].v, sn.v, cs.v, ALU.mult)
            k.tt(dve, tmp[5].v, sn.v, sn.v, ALU.mult)
            k.ts(dve, sn.v, tmp[4].v, 2.0, ALU.mult)
            k.ts(dve, cs.v, tmp[5].v, -2.0, ALU.mult, 1.0, ALU.add)
        PWp = sbt("PWp", [128, 2, 9, G])
        PWn = sbt("PWn", [128, 2, 8, G])
        k.memset(dve, PWp[:, 0, 0, :], 1.0)
        k.memset(dve, PWp[:, 1, 0, :], 0.0)
        k.memset(dve, PWn[:, 0, 0, :], 1.0)
        k.memset(dve, PWn[:, 1, 0, :], 0.0)
        k.tt(dve, PWp[:, 0, 1, :], mag.v, cs.v, ALU.mult)
        k.tt(dve, PWp[:, 1, 1, :], mag.v, sn.v, ALU.mult)
        ctmp = [sbt("ctmp%d" % i, [128, 8, G]) for i in range(2)]

        def cmul(PW, dst, src, n, mk):
            ar, ai = PW[:, 0, src:src + n, :], PW[:, 1, src:src + n, :]
            br = PW[:, 0, mk:mk + 1, :].bc([128, n, G])
            bi = PW[:, 1, mk:mk + 1, :].bc([128, n, G])
            t0, t1 = ctmp[0][:, 0:n, :], ctmp[1][:, 0:n, :]
            k.tt(dve, t0, ar, br, ALU.mult)
            k.tt(dve, t1, ai, bi, ALU.mult)
            k.tt(dve, PW[:, 0, dst:dst + n, :], t0, t1, ALU.subtract)
            k.tt(dve, t0, ar, bi, ALU.mult)
            k.tt(dve, t1, ai, br, ALU.mult)
            k.tt(dve, PW[:, 1, dst:dst + n, :], t0, t1, ALU.add)

        cmul(PWp, 2, 1, 1, 1)
        cmul(PWp, 3, 1, 2, 2)
        cmul(PWp, 5, 1, 4, 4)
        k.tt(dve, tmp[4].v, PWp[:, 0, 1, :], PWp[:, 0, 1, :], ALU.mult)
        k.tt(dve, tmp[5].v, PWp[:, 1, 1, :], PWp[:, 1, 1, :], ALU.mult)
        k.tt(dve, tmp[4].v, tmp[4].v, tmp[5].v, ALU.add)
        k.recip(tmp[5].v, tmp[4].v)
        k.tt(dve, PWn[:, 0, 1, :], PWp[:, 0, 1, :], tmp[5].v, ALU.mult)
        k.stt(dve, PWn[:, 1, 1, :], PWp[:, 1, 1, :], -1.0, tmp[5].v, ALU.mult, ALU.mult)
        cmul(PWn, 2, 1, 1, 1)
        cmul(PWn, 3, 1, 2, 2)
        cmul(PWn, 5, 1, 3, 4)
        k.cp(dve, mu.v, PWp[:, :, 8, :])
        Qm = sbt("Qm", [128, 2, 16, G])
        k.cp(dve, Qm[:, :, 0, :], PWp[:, :, 8, :])
        cmul(Qm, 1, 0, 1, 0)
        cmul(Qm, 2, 0, 2, 1)
        cmul(Qm, 4, 0, 4, 3)
        cmul(Qm, 8, 0, 8, 7)
        for ri in range(2):
            k.cp(pool, PT[F_q, ri], Qm[F_q, ri].re("p k g -> p g k"))
            for kk2 in range(16):
                k.cp(pool, PT[B_q, ri, :, kk2], Qm[B_q, ri, 15 - kk2, :])
        k.cp(dve, mu16[:, 0:2, :], Qm[:, :, 15, :])
        k.ts(dve, mu16[:, 2, :], Qm[:, 1, 15, :], -1.0, ALU.mult)
        if stage == 32:
            for nm, t_ in (("dtt", dtt), ("er", er), ("th", th), ("mag", mag), ("sn", sn), ("cs", cs)):
                k.dma(dout("dbg_" + nm, [128, G]), t_.v, is_output=True)
            k.dma(dout("dbg_PWp", [128, 2 * 9 * G]), PWp.v.re("p a b g -> p (a b g)"), is_output=True)
            k.dma(dout("dbg_PWn", [128, 2 * 8 * G]), PWn.v.re("p a b g -> p (a b g)"), is_output=True)
            k.finish()
            return k
        nr = tmp[0]
        k.ts(dve, nr.v, PWp[:, 0, 1, :], -1.0, ALU.add)
        li = PWp[:, 1, 1, :]
        k.tt(dve, tmp[1].v, aD[:, 0, :], aD[:, 0, :], ALU.mult)
        k.tt(dve, tmp[2].v, aD[:, 1, :], aD[:, 1, :], ALU.mult)
        k.tt(dve, tmp[1].v, tmp[1].v, tmp[2].v, ALU.add)
        k.recip(tmp[2].v, tmp[1].v)
        cfr, cfi = tmp[6], tmp[7]
        k.tt(dve, tmp[3].v, nr.v, aD[:, 0, :], ALU.mult)
        k.tt(dve, tmp[4].v, li, aD[:, 1, :], ALU.mult)
        k.tt(dve, tmp[3].v, tmp[3].v, tmp[4].v, ALU.add)
        k.tt(dve, cfr.v, tmp[3].v, tmp[2].v, ALU.mult)
        k.tt(dve, tmp[3].v, li, aD[:, 0, :], ALU.mult)
        k.tt(dve, tmp[4].v, nr.v, aD[:, 1, :], ALU.mult)
        k.tt(dve, tmp[3].v, tmp[3].v, tmp[4].v, ALU.subtract)
        k.tt(dve, cfi.v, tmp[3].v, tmp[2].v, ALU.mult)
        Bb = sbt("Bb", [128, 2, G, H])
        bt0 = sbt("bt0", [128, G, H])
        bt1 = sbt("bt1", [128, G, H])
        cfr_b = cfr.v.re("p (g o) -> p g o", o=1).bc([128, G, H])
        cfi_b = cfi.v.re("p (g o) -> p g o", o=1).bc([128, G, H])
        k.tt(dve, bt0.v, bD[:, 0], cfr_b, ALU.mult)
        k.tt(dve, bt1.v, bD[:, 1], cfi_b, ALU.mult)
        k.tt(dve, Bb[:, 0], bt0.v, bt1.v, ALU.subtract)
        k.tt(dve, bt0.v, bD[:, 1], cfr_b, ALU.mult)
        k.tt(dve, bt1.v, bD[:, 0], cfi_b, ALU.mult)
        k.tt(dve, Bb[:, 1], bt0.v, bt1.v, ALU.add)
        EW = sbt("EW", [128, 2, G, 8])
        EV = sbt("EV", [128, 2, G, 8])
        EC = sbt("EC", [128, 2, G, 8])
        F_, B_ = slice(0, 64), slice(64, 128)
        for ri in range(2):
            k.cp(act, EW[B_, ri], PWp[B_, ri, 0:8, :].re("p k g -> p g k"))
            k.cp(act, EV[F_, ri], PWp[F_, ri, 1:9, :].re("p k g -> p g k"))
            k.cp(act, EC[B_, ri], PWn[B_, ri, 0:8, :].re("p k g -> p g k"))
            for r in range(8):
                k.cp(act, EW[F_, ri, :, r], PWp[F_, ri, 7 - r, :])
                k.cp(pool, EV[B_, ri, :, r], PWp[B_, ri, 8 - r, :])
                k.cp(act, EC[F_, ri, :, r], PWn[F_, ri, 7 - r, :])
        if stage == 14:
            dd = dout("dbg_mu", [128, 2 * G])
            k.dma(dd, mu.v.re("p a g -> p (a g)"), is_output=True)
            dd2 = dout("dbg_Bb", [128, 2 * G * H])
            k.dma(dd2, Bb.v.re("p a g h -> p (a g h)"), is_output=True)
            dd3 = dout("dbg_EW", [128, 2 * G * 8])
            k.dma(dd3, EW.v.re("p a g r -> p (a g r)"), is_output=True)
            k.finish()
            return k
        GH = G // 2
        big = [sbt("big%d" % i, [128, GH, 8, H]) for i in range(5)]
        sh4 = [128, GH, 8, H]

        def outer(dr, di, E, X, neg_im, gs):
            er_ = E[:, 0, gs].re("p g (r o) -> p g r o", o=1).bc(sh4)
            ei_ = E[:, 1, gs].re("p g (r o) -> p g r o", o=1).bc(sh4)
            xr_ = X[:, 0, gs].re("p g (o h) -> p g o h", o=1).bc(sh4)
            xi_ = X[:, 1, gs].re("p g (o h) -> p g o h", o=1).bc(sh4)
            k.tt(dve, dr.v, er_, xr_, ALU.mult)
            k.tt(pool, di.v, ei_, xi_, ALU.mult)
            k.tt(dve, dr.v, dr.v, di.v, ALU.subtract)
            k.tt(dve, di.v, er_, xi_, ALU.mult)
            k.tt(dve, big[4].v, ei_, xr_, ALU.mult)
            if neg_im:
                k.stt(dve, di.v, di.v, -1.0, big[4].v, ALU.mult, ALU.subtract)
            else:
                k.tt(dve, di.v, di.v, big[4].v, ALU.add)

        Wr, Wi, Cr, Ci = big[0], big[1], big[2], big[3]
        kmA = [sbt("kmA%d" % i, [128, 128]) for i in range(2)]
        kmB = [sbt("kmB%d" % i, [128, 128]) for i in range(2)]
        for gh in range(2):
            gs = slice(gh * GH, (gh + 1) * GH)
            outer(Wr, Wi, EW, Bb, False, gs)
            outer(Cr, Ci, EC, cD, True, gs)
            for gl in range(GH):
                g = gh * GH + gl
                wr = Wr[:, gl].re("p r h -> p (r h)")
                wi = Wi[:, gl].re("p r h -> p (r h)")
                cr = Cr[:, gl].re("p r h -> p (r h)")
                ci = Ci[:, gl].re("p r h -> p (r h)")
                pk = k.ps()
                pk2 = k.ps()
                k.mm(pk[:, 0:128], wr[F_], cr[F_], start=True, stop=False)
                k.mm(pk[:, 0:128], wi[F_], ci[F_], start=False, stop=True)
                k.mm(pk2[:, 0:128], wr[B_], cr[B_], start=True, stop=False)
                k.mm(pk2[:, 0:128], wi[B_], ci[B_], start=False, stop=True)
                kA, kB = kmA[gl % 2], kmB[gl % 2]
                k.tt(dve, kA.v, pk[:, 0:128], maskF, ALU.mult)
                k.tt(dve, kB.v, pk2[:, 0:128], maskB, ALU.mult)
                k.tt(pool, Kmat[:, g, :], kA.v, kB.v, ALU.add)
            for gl in range(GH):
                g = gh * GH + gl
                pw_ = k.ps()
                k.tr(pw_[:, 0:128], Wr[:, gl].re("p r h -> p (r h)"), ident, inc=False)
                k.tr(pw_[:, 128:256], Wi[:, gl].re("p r h -> p (r h)"), ident)
                k.cp(act, WT[:, g].re("p a b -> p (a b)"), pw_[:, 0:256])
            outer(Cr, Ci, EV, cD, True, gs)
            k.cp(act, V2[:, gs, 0, :], Cr.v.re("p g r h -> p g (r h)"))
            k.cp(pool, V2[:, gs, 1, :], Ci.v.re("p g r h -> p g (r h)"))
    if stage <= 2:
        dW = dout("dbg_WT", [128, G * 256], BF16)
        dV = dout("dbg_V2", [128, G * 256], BF16)
        dK = dout("dbg_K", [128, G * 128], BF16)
        dmu = dout("dbg_mu", [128, 2 * G])
        k.dma(dW, WT.v.re("p g a b -> p (g a b)"), is_output=True)
        k.dma(dV, V2.v.re("p g a b -> p (g a b)"), is_output=True)
        k.dma(dK, Kmat.v.re("p g b -> p (g b)"), is_output=True)
        k.dma(dmu, mu.v.re("p a g -> p (a g)"), is_output=True)
        k.finish()
        return k


    st_re_d = dout("st_re", [2, 2, G, P])
    st_im_d = dout("st_im", [2, 2, G, P])
    mu2 = k.sb("mu2", [128, 2, G], F32, stack=stA)
    k.ts(dve, mu2[:, 0, :], mu[:, 1, :], -1.0, ALU.mult)
    k.cp(dve, mu2[:, 1, :], mu[:, 1, :])
    F_, B_ = slice(0, 64), slice(64, 128)

    def norm_mod_T(xt, xn, stt_, mv, rs, cond, moff, dst):
        for hh in range(2):
            k.emit(dve, lambda hh=hh: nc.vector.bn_stats(out=stt_[:, hh, :].ap, in_=xt[:, hh * 512:(hh + 1) * 512].ap),
                   [xt.v], [stt_.v])
        k.emit(dve, lambda: nc.vector.bn_aggr(out=mv.v.ap, in_=stt_.v.re("p a b -> p (a b)").ap), [stt_.v], [mv.v])
        k.ts(dve, rs.v, mv[:, 1:2], LN_EPS, ALU.add)
        k.actv(rs.v, rs.v, AF.Sqrt)
        k.recip(rs.v, rs.v)
        k.ts(dve, xn.v, xt.v, mv[:, 0:1], ALU.subtract, rs.v, ALU.mult)
        for hb in range(2):
            pp = k.ps()
            for q in range(4):
                kc = hb * 4 + q
                k.tr(pp[:, q * 128:(q + 1) * 128], xn[:, kc * 128:(kc + 1) * 128], ident, inc=(q == 3))
            for q in range(4):
                kc = hb * 4 + q
                k.actv(dst(kc), pp[:, q * 128:(q + 1) * 128], AF.Identity,
                       scale=modT[:, moff + 8 + kc, cond:cond + 1], bias=modT[:, moff + kc, cond:cond + 1])

    def ln_front(x_rows, L, cond, h_fm, stack, add_pos=None, moff=0, cond_fn=None):
        xts = [k.sb("xt%d" % i, [128, D], F32, stack=stack) for i in range(2)]
        xns = [k.sb("xn%d" % i, [128, D], F32, stack=stack) for i in range(2)]
        stts = [k.sb("bnst%d" % i, [128, 2, 6], F32, stack=stack) for i in range(2)]
        mvs = [k.sb("mv%d" % i, [128, 2], F32, stack=stack) for i in range(2)]
        rss = [k.sb("rs%d" % i, [128, 1], F32, stack=stack) for i in range(2)]
        for tt in range(L // 128):
            xt, xn, stt_, mv, rs = xts[tt % 2], xns[tt % 2], stts[tt % 2], mvs[tt % 2], rss[tt % 2]
            k.dma(xt.v, x_rows[tt * 128:(tt + 1) * 128, :])
            if cond_fn is not None:
                cond = cond_fn(tt)
            if add_pos is not None:
                k.dma(xn.v, add_pos[tt * 128:(tt + 1) * 128, :])
                k.tt(dve, xt.v, xt.v, xn.v, ALU.add)
            hb_ = h_fm[(tt * 128) // 512]
            o_ = (tt * 128) % 512
            norm_mod_T(xt, xn, stt_, mv, rs, cond, moff, lambda kc, hb_=hb_, o_=o_: hb_[:, kc, o_:o_ + 128])

    def s5_front(u_bf, L, nseq, XsFB, U8):
        XsF, XsB = XsFB
        J = L // 8
        NJ = nseq * J
        gpb = 512 // NJ
        for g0 in range(0, G, gpb):
            pb = k.ps()
            for gi in range(gpb):
                g = g0 + gi
                cc, g8 = g // 8, g % 8
                uv = u_bf[:, cc, :].re("p (j r) -> p r j", r=8)
                for r in range(8):
                    k.mm(pb[:, gi * NJ:(gi + 1) * NJ], sel(g8 * 8 + r), uv[:, r, :], start=(r == 0), stop=(r == 7),
                         inc=(r == 7 and gi == gpb - 1))
            k.cp(act, U8[:, g0:g0 + gpb, :].re("p g j -> p (g j)"), pb[:, 0:gpb * NJ])
        for ri in range(2):
            for g0 in range(0, G, gpb):
                pb = k.ps()
                for gi in range(gpb):
                    g = g0 + gi
                    k.mm(pb[:, gi * NJ:(gi + 1) * NJ], WT[:, g, ri, :], U8[:, g, :], inc=(gi == gpb - 1))
                for s in range(nseq):
                    src = pb[:, 0:gpb * NJ].re("p (g s j) -> p g s j", s=nseq, j=J)
                    k.cp(act, XsF[F_, ri, g0:g0 + gpb, s, 1:J + 1], src[F_, :, s, :])
                    k.cp(dve, XsB[B_, ri, g0:g0 + gpb, s, 0:J], src[B_, :, s, :])

    w_in_v = w_in_d.rearrange("(kc p) n -> p kc n", p=128)

    def proj_front(h_fm, L, nseq, stack, u_bf, vx1, x2, blocks, tok0=0):
        NT = nseq * L
        wb = [k.sb("wblk%d" % i, [128, 8, 512], F32R, stack=stack) for i in range(2)]
        pads = [k.sb("pad%d" % i, [128, nseq, L + 2], F32, stack=stack) for i in range(2 if 0 in blocks else 0)]
        acc = [k.sb("cacc%d" % i, [128, nseq, L], F32, stack=stack) for i in range(1 if 0 in blocks else 0)]
        stg = [k.sb("cstg%d" % i, [128, nseq, L], BF16, stack=stack) for i in range(2 if 0 in blocks else 0)]
        for pd in pads:
            k.memset(pool, pd[:, :, 0:1], 0.0)
            k.memset(pool, pd[:, :, L + 1:L + 2], 0.0)
        ci = 0
        for bi, b in enumerate(blocks):
            wt = wb[bi % 2]
            k.dma(wt.v, w_in_v[:, :, b * 512:(b + 1) * 512])
            for q in range(4):
                ch = b * 4 + q
                pbs = []
                for t0 in range(0, NT, 512):
                    n = min(512, NT - t0)
                    pu = k.ps()
                    for kc in range(8):
                        k.mm(pu[:, 0:n], wt[:, kc, q * 128:(q + 1) * 128], h_fm[t0 // 512][:, kc, 0:n], start=(kc == 0), stop=(kc == 7))
                    pbs.append((pu, t0, n))
                if b == 3:
                    for pu, t0, n in pbs:
                        k.cp(act, u_bf[:, q, t0:t0 + n], pu[:, 0:n])
                    continue
                pd, ac = pads[ci % 2], acc[0]
                ci += 1
                for pu, t0, n in pbs:
                    if L >= 512:
                        s = t0 // L
                        l0 = t0 % L
                        k.cp(act, pd[:, s, 1 + l0:1 + l0 + n], pu[:, 0:n])
                    else:
                        k.cp(act, pd[:, t0 // L:(t0 + n) // L, 1:L + 1], pu[:, 0:n].re("p (s l) -> p s l", l=L))
                eng = dve if ch % 2 == 0 else pool
                w0 = vecT[:, 24 + ch:25 + ch]
                w1 = vecT[:, 36 + ch:37 + ch]
                w2 = vecT[:, 48 + ch:49 + ch]
                bb = vecT[:, 60 + ch:61 + ch]
                k.ts(eng, ac.v, pd[:, :, 0:L], w0, ALU.mult, bb, ALU.add)
                k.stt(dve, ac.v, pd[:, :, 1:L + 1], w1, ac.v, ALU.mult, ALU.add)
                sg = stg[ci % 2]
                k.stt(dve, sg.v, pd[:, :, 2:L + 2], w2, ac.v, ALU.mult, ALU.add)
                k.dma(vx_t[ch, :, tok0:tok0 + NT], sg.v.re("p s l -> p (s l)"))

    def s5_scan_seq(XsFB, J, nseq, stack):
        tA = [k.sb("scA%d" % i, [128, 2, G, nseq], F32, stack=stack) for i in range(2)]
        tB = [k.sb("scB%d" % i, [128, 2, G, nseq], F32, stack=stack) for i in range(2)]
        shp = [64, 2, G, nseq]
        for j in range(J):
            for di, (eng, half) in enumerate(((dve, F_), (dve, B_))):
                Xs = XsFB[di]
                prev = j if di == 0 else J - j
                cur = j + 1 if di == 0 else J - 1 - j
                Sp = Xs[half, :, :, :, prev]
                m1 = mu[half, 0:1, :].re("p a (g o) -> p a g o", o=1).bc(shp)
                a, b = tA[di], tB[di]
                k.tt(eng, a[half], Sp, m1, ALU.mult)
                k.tt(eng, b[half, 0], Xs[half, 1, :, :, prev], mu2[half, 0, :].re("p (g o) -> p g o", o=1).bc([64, G, nseq]), ALU.mult)
                k.tt(eng, b[half, 1], Xs[half, 0, :, :, prev], mu2[half, 1, :].re("p (g o) -> p g o", o=1).bc([64, G, nseq]), ALU.mult)
                k.tt(eng, a[half], a[half], b[half], ALU.add)
                k.tt(eng, Xs[half, :, :, :, cur], Xs[half, :, :, :, cur], a[half], ALU.add)


    def s5_scan(XsFB, J, nseq, stack):
        Jg = 16
        nseg = J // Jg
        tA0 = k.sb("scA", [128, 2, G, nseg], F32, stack=stack)
        tB0 = k.sb("scB", [128, 2, G, nseg], F32, stack=stack)
        Cc0 = k.sb("scC", [128, 2, G, nseg], F32, stack=stack)
        u10 = k.sb("scU1", [128, G, nseg, Jg], F32, stack=stack)
        u20 = k.sb("scU2", [128, G, nseg, Jg], F32, stack=stack)
        tA = (tA0, k.alias(tA0, "scAb", stack)); tB = (tB0, k.alias(tB0, "scBb", stack))
        Cc = (Cc0, k.alias(Cc0, "scCb", stack)); u1 = (u10, k.alias(u10, "scU1b", stack)); u2 = (u20, k.alias(u20, "scU2b", stack))
        for s in range(nseq):
            for di, (eng, half) in enumerate(((dve, F_), (dve, B_))):
                Xs = XsFB[di]
                c0 = 1 if di == 0 else 0
                Xv = Xs[half, :, :, s, c0:c0 + J].re("p a g (q k) -> p a g q k", k=Jg)
                a, b, C, w1, w2 = tA[di], tB[di], Cc[di], u1[di], u2[di]
                m1 = mu[half, 0:1, :].re("p a (g o) -> p a g o", o=1).bc([64, 2, G, nseg])
                m2a = mu2[half, 0, :].re("p (g o) -> p g o", o=1).bc([64, G, nseg])
                m2b = mu2[half, 1, :].re("p (g o) -> p g o", o=1).bc([64, G, nseg])
                for step in range(1, Jg):
                    kc_ = step if di == 0 else Jg - 1 - step
                    kp_ = kc_ - 1 if di == 0 else kc_ + 1
                    k.tt(eng, a[half], Xv[:, :, :, :, kp_], m1, ALU.mult)
                    k.tt(eng, b[half, 0], Xv[:, 1, :, :, kp_], m2a, ALU.mult)
                    k.tt(eng, b[half, 1], Xv[:, 0, :, :, kp_], m2b, ALU.mult)
                    k.tt(eng, a[half], a[half], b[half], ALU.add)
                    k.tt(eng, Xv[:, :, :, :, kc_], Xv[:, :, :, :, kc_], a[half], ALU.add)
                h0col = Xs[half, :, :, s, 0] if di == 0 else Xs[half, :, :, s, J]
                order = list(range(nseg)) if di == 0 else list(range(nseg - 1, -1, -1))
                k.cp(eng, C[half, :, :, order[0]], h0col)
                kend = Jg - 1 if di == 0 else 0
                r16 = mu16[half, 0:1, :].bc([64, 2, G])
                for ii in range(nseg - 1):
                    sg, nx = order[ii], order[ii + 1]
                    Cs = C[half, :, :, sg]
                    k.tt(eng, a[half, :, :, 0], Cs, r16, ALU.mult)
                    k.tt(eng, b[half, 0, :, 0], C[half, 1, :, sg], mu16[half, 2, :], ALU.mult)
                    k.tt(eng, b[half, 1, :, 0], C[half, 0, :, sg], mu16[half, 1, :], ALU.mult)
                    k.tt(eng, a[half, :, :, 0], a[half, :, :, 0], b[half, :, :, 0], ALU.add)
                    k.tt(eng, C[half, :, :, nx], Xv[:, :, :, sg, kend], a[half, :, :, 0], ALU.add)
                sh = [64, G, nseg, Jg]
                pr = PT[half, 0].re("p g (o k) -> p g o k", o=1).bc(sh)
                pi_ = PT[half, 1].re("p g (o k) -> p g o k", o=1).bc(sh)
                cr = C[half, 0].re("p g (q o) -> p g q o", o=1).bc(sh)
                ci = C[half, 1].re("p g (q o) -> p g q o", o=1).bc(sh)
                k.tt(eng, w1[half], pr, cr, ALU.mult)
                k.tt(eng, w2[half], pi_, ci, ALU.mult)
                k.tt(eng, w1[half], w1[half], w2[half], ALU.subtract)
                k.tt(eng, Xv[:, 0], Xv[:, 0], w1[half], ALU.add)
                k.tt(eng, w1[half], pr, ci, ALU.mult)
                k.tt(eng, w2[half], pi_, cr, ALU.mult)
                k.tt(eng, w1[half], w1[half], w2[half], ALU.add)
                k.tt(eng, Xv[:, 1], Xv[:, 1], w1[half], ALU.add)

    wglu = k.sb("wglu", [128, 4, 512], F32R, stack=stA)
    k.dma(wglu.v, w_glu_d.rearrange("(kc p) n -> p kc n", p=128))
    GC = 2.0 * math.sqrt(2.0 / math.pi)

    def s5_back(XsFB, U8, u_bf, L, nseq, stack, y_fm):
        XsF, XsB = XsFB
        J = L // 8
        NJ = nseq * J
        gpb = 512 // NJ
        Xprev = k.sb("Xprev", [128, 2, G, nseq, J], BF16, stack=stack)
        for ri in range(2):
            k.cp(dve, Xprev[F_, ri], XsF[F_, ri, :, :, 0:J])
            k.cp(act, Xprev[B_, ri], XsB[B_, ri, :, :, 1:J + 1])
        y8 = k.sb("y8", [128, G, NJ], BF16, stack=stack)
        for g0 in range(0, G, gpb):
            pb = k.ps()
            for gi in range(gpb):
                g = g0 + gi
                o = pb[:, gi * NJ:(gi + 1) * NJ]
                k.mm(o, Kmat[:, g, :], U8[:, g, :], start=True, stop=False)
                k.mm(o, V2[:, g, 0, :], Xprev[:, 0, g].re("p s j -> p (s j)"), start=False, stop=False)
                k.mm(o, V2[:, g, 1, :], Xprev[:, 1, g].re("p s j -> p (s j)"), start=False, stop=True, inc=(gi == gpb - 1))
            k.cp(act, y8[:, g0:g0 + gpb, :].re("p g j -> p (g j)"), pb[:, 0:gpb * NJ])
        rpb = min(8, 512 // NJ)
        for cc in range(4):
            yv = y_fm[:, cc, :].re("p (sj r) -> p r sj", r=8)
            uv = u_bf[:, cc, :].re("p (sj r) -> p r sj", r=8)
            for r0 in range(0, 8, rpb):
                pb = k.ps()
                for rr in range(rpb):
                    r = r0 + rr
                    for g8 in range(8):
                        k.mm(pb[:, rr * NJ:(rr + 1) * NJ], sel(r * 8 + g8), y8[:, cc * 8 + g8, :], start=(g8 == 0), stop=(g8 == 7),
                             inc=(g8 == 7 and rr == rpb - 1))
                for rr in range(rpb):
                    r = r0 + rr
                    k.stt(dve, yv[:, r, :], uv[:, r, :], vecT[:, 72 + cc:73 + cc], pb[:, rr * NJ:(rr + 1) * NJ], ALU.mult, ALU.add)

    def glu_rms(y_fm, NT, stack, mixed_fm, moff, gcol):
        y_r = k.sb("y_r", [128, 4, NT], F32R, stack=stack)
        k.cp(act, y_r.v, y_fm.v)
        ys5 = k.sb("ys5", [128, 4, NT], F32, stack=stack)
        sg = [k.sb("sg%d" % i, [128, 512], F32, stack=stack) for i in range(2)]
        ge = k.sb("ge", [128, 4, NT], F32, stack=stack)
        k.tt(dve, ge.v, y_fm.v, y_fm.v, ALU.mult)
        k.ts(dve, ge.v, ge.v, 0.044715, ALU.mult, 1.0, ALU.add)
        k.tt(dve, ge.v, ge.v, y_fm.v, ALU.mult)
        k.actv(ge.v, ge.v, AF.Sigmoid, scale=GC)
        k.tt(dve, ge.v, ge.v, y_fm.v, ALU.mult)
        bi = 0
        for nch in range(4):
            for t0 in range(0, NT, 512):
                n = min(512, NT - t0)
                pg = k.ps()
                for cc in range(4):
                    k.mm(pg[:, 0:n], wglu[:, cc, nch * 128:(nch + 1) * 128], y_r[:, cc, t0:t0 + n], start=(cc == 0), stop=(cc == 3))
                s_ = sg[bi % 2]
                bi += 1
                k.actv(s_[:, 0:n], pg[:, 0:n], AF.Sigmoid, bias=vecT[:, 76 + nch:77 + nch])
                k.tt(dve, ys5[:, nch, t0:t0 + n], ge[:, nch, t0:t0 + n], s_[:, 0:n], ALU.mult)
        rms_mix(ys5, NT, stack, mixed_fm, moff, gcol)
        return ys5

    def rms_mix(yy, NT, stack, mixed_fm, moff, gcol):
        sq = k.sb("sq", [128, 4, NT], BF16, stack=stack)
        k.actv(sq.v, yy if isinstance(yy, V) else yy.v, AF.Square)
        rbc = k.sb("rbc", [128, 512], F32, stack=stack)
        for t0 in range(0, NT, 512):
            n = min(512, NT - t0)
            pr = k.ps()
            for cc in range(4):
                k.mm(pr[:, 0:n], ones_b, sq[:, cc, t0:t0 + n], start=(cc == 0), stop=(cc == 3))
            k.actv(rbc[:, 0:n], pr[:, 0:n], AF.Sqrt, scale=1.0 / 512.0, bias=epsc.v)
            k.recip(rbc[:, 0:n], rbc[:, 0:n])
            for cc in range(4):
                k.stt(dve, mixed_fm[:, moff + cc, t0:t0 + n], yy[:, cc, t0:t0 + n], vecT[:, gcol + cc:gcol + cc + 1],
                      rbc[:, 0:n], ALU.mult, ALU.mult)


    def to_tm(dst, srcv, L, s):
        for tt in range(L // 128):
            pb = k.ps()
            pbb = pb.v.bitcast(BF16)
            for cc in range(4):
                k.tr(pbb[:, cc * 128:(cc + 1) * 128], srcv[:, cc, s * L + tt * 128: s * L + (tt + 1) * 128], ident_b, inc=(cc == 3))
            k.cp(act, dst[:, tt, :], pbb[:, 0:512])

    def hyena_conv(L, nseq, Ktab, DF, vtm_all, x1tm_all, x2, stack, yhy):
        nf = L // 128
        Ysp = k.sb("Ysp", [128, nf, 2, 512], BF16, stack=stack)
        ta = [k.sb("hta%d" % i, [128, 512], F32, stack=stack) for i in range(4)]

        def fwd_mul(src_tm, o):
            for ft in range(nf):
                pU = k.ps(); pV = k.ps()
                for s_ in range(nf):
                    k.mm(pU.v, DF[:, 0, s_, ft * 128:(ft + 1) * 128], src_tm[:, s_, :], start=(s_ == 0), stop=(s_ == nf - 1))
                for s_ in range(nf):
                    k.mm(pV.v, DF[:, 1, s_, ft * 128:(ft + 1) * 128], src_tm[:, s_, :], start=(s_ == 0), stop=(s_ == nf - 1))
                KA = Ktab[:, ft, 0, o, :]
                KB_ = Ktab[:, ft, 1, o, :]
                k.tt(dve, ta[0].v, pU.v, KA, ALU.mult)
                k.tt(dve, ta[1].v, pV.v, KB_, ALU.mult)
                k.tt(dve, Ysp[:, ft, 0, :], ta[0].v, ta[1].v, ALU.add)
                k.tt(dve, ta[2].v, pV.v, KA, ALU.mult)
                k.tt(dve, ta[3].v, pU.v, KB_, ALU.mult)
                k.tt(pool, Ysp[:, ft, 1, :], ta[2].v, ta[3].v, ALU.subtract)

        for s in range(nseq):
            v_tm = vtm_all[:, s]
            x1_tm = x1tm_all[:, s]
            z_tm = v_tm
            fwd_mul(v_tm, 0)
            for tt in range(nf):
                pz = k.ps()
                for ft in range(nf):
                    k.mm(pz.v, DF[:, 0, ft, tt * 128:(tt + 1) * 128], Ysp[:, ft, 0, :], start=(ft == 0), stop=False)
                    k.mm(pz.v, DF[:, 1, ft, tt * 128:(tt + 1) * 128], Ysp[:, ft, 1, :], start=False, stop=(ft == nf - 1))
                k.tt(dve, z_tm[:, tt, :], pz.v, x1_tm[:, tt, :], ALU.mult)
            fwd_mul(z_tm, 1)
            for cc in range(4):
                for t0 in range(0, L, 512):
                    n = min(512, L - t0)
                    py = k.ps()
                    for ft in range(nf):
                        k.mm(py[:, 0:n], Ysp[:, ft, 0, cc * 128:(cc + 1) * 128], DF[:, 0, ft, t0:t0 + n], start=(ft == 0), stop=False)
                        k.mm(py[:, 0:n], Ysp[:, ft, 1, cc * 128:(cc + 1) * 128], DF[:, 1, ft, t0:t0 + n], start=False, stop=(ft == nf - 1))
                    k.tt(dve, yhy[:, cc, s * L + t0: s * L + t0 + n], py[:, 0:n], x2[:, cc, s * L + t0: s * L + t0 + n], ALU.mult)


    def ln_tile(r_, stack_tiles, gk, bk, out_v):
        stt_, mv, rs, xn = stack_tiles
        for hh in range(2):
            k.emit(dve, lambda hh=hh: nc.vector.bn_stats(out=stt_[:, hh, :].ap, in_=r_[:, hh * 512:(hh + 1) * 512].ap), [r_], [stt_.v])
        k.emit(dve, lambda: nc.vector.bn_aggr(out=mv.v.ap, in_=stt_.v.re("p a b -> p (a b)").ap), [stt_.v], [mv.v])
        k.ts(dve, rs.v, mv[:, 1:2], LN_EPS, ALU.add)
        k.actv(rs.v, rs.v, AF.Sqrt)
        k.recip(rs.v, rs.v)
        k.ts(dve, xn.v, r_, mv[:, 0:1], ALU.subtract, rs.v, ALU.mult)
        k.tt(dve, xn.v, xn.v, gk.v, ALU.mult)
        k.tt(pool, out_v, xn.v, bk.v, ALU.add)

    def out_proj_ln1(mixH_, mixS_, x_rows, NT, cond, stack, row0, add_pos=None):
        wo = k.sb("wo", [128, 8, D], F32R, stack=stack)
        k.dma(wo.v, w_out_d.rearrange("(kc p) n -> p kc n", p=128))
        xts = [k.sb("oxt%d" % i, [128, D], F32, stack=stack) for i in range(2)]
        tmps = [k.sb("otmp%d" % i, [128, D], F32, stack=stack) for i in range(2)]
        x1o = [k.sb("x1o%d" % i, [128, D], F32, stack=stack) for i in range(2)]
        h2stg = [k.sb("h2stg%d" % i, [128, 8, 128], F32R, stack=stack) for i in range(2)]
        tl = [(k.sb("obn%d" % i, [128, 2, 6], F32, stack=stack), k.sb("omv%d" % i, [128, 2], F32, stack=stack),
               k.sb("ors%d" % i, [128, 1], F32, stack=stack), k.sb("oxn%d" % i, [128, D], F32, stack=stack)) for i in range(2)]
        tl2 = [(k.sb("pbn%d" % i, [128, 2, 6], F32, stack=stack), k.sb("pmv%d" % i, [128, 2], F32, stack=stack),
                k.sb("prs%d" % i, [128, 1], F32, stack=stack)) for i in range(2)]
        gb, lb = load_bc(stack, 0, [cond], ("ln1_g", "ln1_b"))
        ntl = NT // 128

        def stage1(tt):
            xt, tmp, tiles = xts[tt % 2], tmps[tt % 2], tl[tt % 2]
            k.dma(xt.v, x_rows[tt * 128:(tt + 1) * 128, :])
            if add_pos is not None:
                k.dma(tmp.v, add_pos[tt * 128:(tt + 1) * 128, :])
                k.tt(dve, xt.v, xt.v, tmp.v, ALU.add)
            pos_ = []
            for hb in range(2):
                po = k.ps()
                for kc in range(8):
                    lh = mixH_[:, kc, tt * 128:(tt + 1) * 128] if kc < 4 else mixS_[:, kc - 4, tt * 128:(tt + 1) * 128]
                    k.mm(po.v, lh, wo[:, kc, hb * 512:(hb + 1) * 512], start=(kc == 0), stop=(kc == 7))
                pos_.append(po)
            for hb in range(2):
                k.tt(dve, tmp[:, hb * 512:(hb + 1) * 512], pos_[hb].v, gb[cond][:, hb * 512:(hb + 1) * 512], ALU.mult)
            k.stt(dve, tmp.v, xt.v, ALPHA, tmp.v, ALU.mult, ALU.add)
            xo_ = x1o[tt % 2]
            ln_tile(tmp.v, tiles, lb["ln1_g"], lb["ln1_b"], xo_.v)
            k.dma(x1_t[row0 + tt * 128: row0 + (tt + 1) * 128, :], xo_.v)

        def stage2(tt):
            tmp, t2 = tmps[tt % 2], tl2[tt % 2]
            xo_ = x1o[tt % 2]
            h2s = h2stg[tt % 2]
            norm_mod_T(xo_, tmp, t2[0], t2[1], t2[2], cond, 24, lambda kc, h2s=h2s: h2s[:, kc, :])
            k.dma(h2_t[:, :, row0 + tt * 128: row0 + (tt + 1) * 128], h2s.v)

        for tt in range(ntl + 1):
            if tt < ntl:
                stage1(tt)
            if tt >= 1:
                stage2(tt - 1)

    BIG = 1.0e30

    def moe_half(hh, stack):
        NTH = NTOK
        NTL = NTH // 128
        TB = 512
        h2 = k.sb("h2", [128, 8, NTH], F32R, stack=stack)
        k.dma(h2.v, h2_t[:, :, hh * NTH:(hh + 1) * NTH])
        wr = k.sb("wr", [128, 8, 20], F32R, stack=stack)
        k.dma(wr.v, wr_d.rearrange("(kc p) n -> p kc n", p=128))
        brb = k.sb("brb", [128, 20], F32, stack=stack)
        k.dma(brb.v, br_d.broadcast_to([128, 20]))
        gates = k.sb("gates", [128, NTL, 16], F32, stack=stack)
        with k.scope() as st3:
            sm = lambda n, s: k.sb(n, s, F32, stack=st3)
            T_ = NTL
            lg = sm("lg", [128, T_, 20]); m1 = sm("m1", [128, T_]); d1 = sm("d1", [128, T_, 4]); e1 = sm("e1", [128, T_, 4])
            s1 = sm("s1", [128, T_]); pg = sm("pg", [128, T_]); oh = sm("oh", [128, T_, 4]); l2m = sm("l2m", [128, T_, 16])
            mx1 = sm("mx1", [128, T_]); oh1 = sm("oh1", [128, T_, 16]); l2b = sm("l2b", [128, T_, 16]); mx2 = sm("mx2", [128, T_])
            oh2 = sm("oh2", [128, T_, 16]); dd = sm("dd", [128, T_]); w1_ = sm("w1_", [128, T_]); w2_ = sm("w2_", [128, T_])
            gt = sm("gt", [128, T_, 16])
            pl = k.ps()
            for tt in range(T_):
                for kc in range(8):
                    k.mm(pl[:, tt * 20:(tt + 1) * 20], h2[:, kc, tt * 128:(tt + 1) * 128], wr[:, kc, :], start=(kc == 0), stop=(kc == 7),
                         inc=(kc == 7 and tt == T_ - 1))
            bcl = lambda v_, n_: v_.re("p (t o) -> p t o", o=1).bc([128, T_, n_])
            k.tt(dve, lg.v, pl[:, 0:T_ * 20].re("p (t c) -> p t c", c=20), brb.v.re("p (o c) -> p o c", o=1).bc([128, T_, 20]), ALU.add)
            k.reduce(m1.v, lg[:, :, 0:4], ALU.max)
            k.tt(dve, d1.v, lg[:, :, 0:4], bcl(m1.v, 4), ALU.subtract)
            k.actv(e1.v, d1.v, AF.Exp)
            k.reduce(s1.v, e1.v, ALU.add)
            k.recip(pg.v, s1.v)
            k.ts(dve, oh.v, d1.v, 0.0, ALU.is_equal)
            k.ts(dve, oh.v, oh.v, BIG, ALU.mult, -BIG, ALU.add)
            for g_i in range(4):
                k.tt(dve, l2m[:, :, g_i * 4:(g_i + 1) * 4], lg[:, :, 4 + g_i * 4:8 + g_i * 4],
                     oh[:, :, g_i:g_i + 1].bc([128, T_, 4]), ALU.add)
            k.reduce(mx1.v, l2m.v, ALU.max)
            k.tt(dve, oh1.v, l2m.v, bcl(mx1.v, 16), ALU.is_equal)
            k.stt(dve, l2b.v.re("p t c -> p (t c)"), oh1.v.re("p t c -> p (t c)"), -BIG, l2m.v.re("p t c -> p (t c)"), ALU.mult, ALU.add)
            k.reduce(mx2.v, l2b.v, ALU.max)
            k.tt(dve, oh2.v, l2b.v, bcl(mx2.v, 16), ALU.is_equal)
            k.tt(dve, dd.v, mx2.v, mx1.v, ALU.subtract)
            k.actv(dd.v, dd.v, AF.Exp)
            k.ts(dve, w1_.v, dd.v, 1.0, ALU.add)
            k.recip(w1_.v, w1_.v)
            k.tt(dve, w2_.v, dd.v, w1_.v, ALU.mult)
            k.tt(dve, w1_.v, w1_.v, pg.v, ALU.mult)
            k.tt(dve, w2_.v, w2_.v, pg.v, ALU.mult)
            k.tt(dve, gt.v, oh1.v, bcl(w1_.v, 16), ALU.mult)
            k.tt(dve, oh2.v, oh2.v, bcl(w2_.v, 16), ALU.mult)
            k.tt(dve, gates.v, gt.v, oh2.v, ALU.add)
        y_acc = k.sb("y_acc", [128, NTL, D], F32, stack=stack)
        _scw = k.scope()
        stack = _scw.__enter__()
        wg = [k.sb("wg%d" % i, [128, 8, 512], F32R, stack=stack) for i in range(2)]
        wu = [k.sb("wu%d" % i, [128, 8, 512], F32R, stack=stack) for i in range(2)]
        wd = [k.sb("wd%d" % i, [128, 4, D], F32R, stack=stack) for i in range(1)]
        hids = [k.sb("hid%d" % i, [128, 4, TB], F32R, stack=stack) for i in range(2)]
        sa = [k.sb("sa%d" % i, [128, TB], F32, stack=stack) for i in range(2)]
        for e in range(16):
            g_, u_, d_ = wg[e % 2], wu[e % 2], wd[0]
            k.dma(g_.v, wgate_d[e].rearrange("(kc p) n -> p kc n", p=128))
            k.dma(u_.v, wup_d[e].rearrange("(kc p) n -> p kc n", p=128))
            k.dma(d_.v, wdown_d[e].rearrange("(kc p) n -> p kc n", p=128))
            def gu(tb):
                hd_ = hids[(tb // TB) % 2]
                for fc in range(4):
                    pa = k.ps(); pu = k.ps()
                    for kc in range(8):
                        k.mm(pa[:, 0:TB], g_[:, kc, fc * 128:(fc + 1) * 128], h2[:, kc, tb:tb + TB], start=(kc == 0), stop=(kc == 7))
                    for kc in range(8):
                        k.mm(pu[:, 0:TB], u_[:, kc, fc * 128:(fc + 1) * 128], h2[:, kc, tb:tb + TB], start=(kc == 0), stop=(kc == 7))
                    s_ = sa[fc % 2]
                    k.actv(s_.v, pa[:, 0:TB], AF.Silu)
                    k.tt(dve, hd_[:, fc, :], s_.v, pu[:, 0:TB], ALU.mult)

            def dn(tb):
                hd_ = hids[(tb // TB) % 2]
                for t3 in range(TB // 128):
                    tt = tb // 128 + t3
                    for nb in range(2):
                        py = k.ps()
                        for fc in range(4):
                            k.mm(py.v, hd_[:, fc, t3 * 128:(t3 + 1) * 128], d_[:, fc, nb * 512:(nb + 1) * 512], start=(fc == 0), stop=(fc == 3))
                        ya = y_acc[:, tt, nb * 512:(nb + 1) * 512]
                        if e == 0:
                            k.ts(dve, ya, py.v, gates[:, tt, e:e + 1], ALU.mult)
                        else:
                            k.stt(dve, ya, py.v, gates[:, tt, e:e + 1], ya, ALU.mult, ALU.add)

            tbs = list(range(0, NTH, TB))
            gu(tbs[0])
            for i_ in range(1, len(tbs)):
                gu(tbs[i_])
                dn(tbs[i_ - 1])
            dn(tbs[-1])
        _scw.__exit__(None, None, None)
        return y_acc

    def moe_final(hh, y_acc, stack):
        NTL = NTOK // 128
        NB = 3
        xts = [k.sb("fxt%d" % i, [128, D], F32, stack=stack) for i in range(NB)]
        tmps = [k.sb("ftmp%d" % i, [128, D], F32, stack=stack) for i in range(NB)]
        yo = [k.sb("fyo%d" % i, [128, D], F32, stack=stack) for i in range(NB)]
        tl = [(k.sb("fbn%d" % i, [128, 2, 6], F32, stack=stack), k.sb("fmv%d" % i, [128, 2], F32, stack=stack),
               k.sb("frs%d" % i, [128, 1], F32, stack=stack)) for i in range(NB)]
        gb, lb = load_bc(stack, 1, [0, 1], ("ln2_g", "ln2_b"))

        def stage_a(tt):
            cond = 0 if tt < 4 else 1
            xt, tmp, (stt_, mv, rs) = xts[tt % NB], tmps[tt % NB], tl[tt % NB]
            k.dma(xt.v, x1_t[tt * 128:(tt + 1) * 128, :])
            k.tt(dve, tmp.v, y_acc[:, tt, :], gb[cond].v, ALU.mult)
            k.stt(dve, tmp.v, xt.v, ALPHA, tmp.v, ALU.mult, ALU.add)
            for h_ in range(2):
                k.emit(dve, lambda h_=h_: nc.vector.bn_stats(out=stt_[:, h_, :].ap, in_=tmp[:, h_ * 512:(h_ + 1) * 512].ap), [tmp.v], [stt_.v])
            k.emit(dve, lambda: nc.vector.bn_aggr(out=mv.v.ap, in_=stt_.v.re("p a b -> p (a b)").ap), [stt_.v], [mv.v])
            k.ts(dve, rs.v, mv[:, 1:2], LN_EPS, ALU.add)
            k.actv(rs.v, rs.v, AF.Sqrt)
            k.recip(rs.v, rs.v)
            k.ts(dve, xt.v, tmp.v, mv[:, 0:1], ALU.subtract, rs.v, ALU.mult)

        def stage_b(tt):
            xt, yo_ = xts[tt % NB], yo[tt % NB]
            k.tt(dve, xt.v, xt.v, lb["ln2_g"].v, ALU.mult)
            k.tt(pool, yo_.v, xt.v, lb["ln2_b"].v, ALU.add)
            if tt < 4:
                k.dma(y_p[tt * 128:(tt + 1) * 128, :], yo_.v, is_output=True)
            else:
                k.dma(y_s[(tt - 4) * 128:(tt - 3) * 128, :], yo_.v, is_output=True)

        for tt in range(NTL + 1):
            if tt < NTL:
                stage_a(tt)
            if tt >= 1:
                stage_b(tt - 1)

    def s5_pass2(x_rows, L, nseq, cond, tok0, pos_rows, use_h0, st_out):
        NT = nseq * L
        J = L // 8
        with k.scope() as st:
            u_bf = k.sb("u_bf", [128, 4, NT], BF16, stack=st)
            with k.scope() as st2:
                h = [k.sb("h_fm%d" % i, [128, 8, 512], F32R, stack=st2) for i in range(NT // 512)]
                with k.scope() as st3:
                    ln_front(x_rows, NT, cond, h, st3, add_pos=pos_rows)
                proj_front(h, L, nseq, st2, u_bf, None, None, (3, 0, 1, 2), tok0=tok0)
            y_fm = k.sb("y_fm", [128, 4, NT], F32, stack=st)
            with k.scope() as stS:
                XsF = k.sb("Xs", [128, 2, G, nseq, J + 1], F32, stack=stS)
                XsB = k.alias(XsF, "XsB", stS)
                Xs = (XsF, XsB)
                U8 = k.sb("U8", [128, G, nseq * J], BF16, stack=stS)
                if use_h0:
                    k.cp(pool, XsF[F_, :, :, 0, 0], h0D[F_])
                    k.cp(dve, XsB[B_, :, :, 0, J], h0D[B_])
                else:
                    k.memset(pool, XsF[F_, :, :, :, 0], 0.0)
                    k.memset(dve, XsB[B_, :, :, :, J], 0.0)
                s5_front(u_bf, L, nseq, Xs, U8)
                if stage == 30 and st_out:
                    pass
                k.mark("scan%d" % L)
                with k.scope() as st2:
                    (s5_scan if J >= 64 else s5_scan_seq)(Xs, J, nseq, st2)
                k.mark("s5_back%d" % L)
                pass
                if st_out:
                    with k.scope() as st2:
                        fin = k.sb("fin", [128, 2, 2, G], F32, stack=st2)
                        for s in range(2):
                            k.cp(pool, fin[F_, s], XsF[F_, :, :, s, J])
                            k.cp(dve, fin[B_, s], XsB[B_, :, :, s, 0])
                        pf = k.ps()
                        for s in range(2):
                            for ri in range(2):
                                i = s * 2 + ri
                                k.tr(pf[0:32, i * 128:(i + 1) * 128], fin[:, s, ri, :], ident, inc=(i == 3))
                        stT = k.sb("stT", [32, 4, 128], F32, stack=st2)
                        k.cp(act, stT.v.re("g a q -> g (a q)"), pf[0:32, :])
                        for s in range(2):
                            k.dma(st_re_d[s].rearrange("d g p -> g d p"), stT[:, s * 2 + 0, :].re("g (d p) -> g d p", d=2), is_output=True)
                            k.dma(st_im_d[s].rearrange("d g p -> g d p"), stT[:, s * 2 + 1, :].re("g (d p) -> g d p", d=2), is_output=True)
                with k.scope() as st2:
                    s5_back(Xs, U8, u_bf, L, nseq, st2, y_fm)
            with k.scope() as st2:
                glu_rms(y_fm, NT, st2, mixS[:, :, tok0:tok0 + NT], 0, gcol=20)

    k.mark("s5_prompts")
    s5_pass2(xp_d, LP, 2, 0, 0, None, False, True)
    if stage in (30, 31):
        return k
    k.mark("s5_sample")
    s5_pass2(xs_d, LS, 1, 1, 2 * LP, pos_d, True, False)
    _scA.__exit__(None, None, None)

    def hy_pass(x_rows, L, nseq, cond, tok0, pos_rows):
        NT = nseq * L
        nf = L // 128
        with k.scope() as st:
            mixH = k.sb("mixH", [128, 4, NT], F32R, stack=st)
            x2 = k.sb("x2", [128, 4, NT], BF16, stack=st)
            vtm = k.sb("vtm", [128, nseq, nf, 512], BF16, stack=st)
            x1tm = k.sb("x1tm", [128, nseq, nf, 512], BF16, stack=st)
            with k.scope() as stV:
                vx1 = k.sb("vx1", [128, 8, NT], BF16, stack=stV)
                for ch in range(12):
                    dst_ = vx1[:, ch, :] if ch < 8 else x2[:, ch - 8, :]
                    k.dma(dst_, vx_t[ch, :, tok0:tok0 + NT])
                for s in range(nseq):
                    to_tm(vtm[:, s], vx1[:, 0:4, :], L, s)
                    to_tm(x1tm[:, s], vx1[:, 4:8, :], L, s)
            with k.scope() as stK:
                Kt = k.sb("Kt", [128, nf, 2, 2, 512], BF16, stack=stK)
                k.dma(Kt.v.re("p a b c d -> p (a b c d)"), kt_t[L].v)
                k.mark("hy_conv%d" % L)
                DF = k.sb("DF", [128, 2, nf, L], BF16, stack=stK)
                k.dma(DF.v, dft_d[L][:, 0:2])
                hyena_conv(L, nseq, Kt, DF, vtm, x1tm, x2, stK, mixH)
            with k.scope() as st2:
                rms_mix(mixH.v.bitcast(F32), NT, st2, mixH.v, 0, gcol=16)
            k.mark("out_proj%d" % L)
            with k.scope() as st2:
                out_proj_ln1(mixH, mixS[:, :, tok0:tok0 + NT], x_rows, NT, cond, st2, tok0, add_pos=pos_rows)

    k.mark("hy_prompts")
    hy_pass(xp_d, LP, 2, 0, 0, None)
    k.mark("hy_sample")
    hy_pass(xs_d, LS, 1, 1, 2 * LP, pos_d)
    _scB.__exit__(None, None, None)
    k.mark("moe")

    with k.scope() as st:
        ya = moe_half(0, st)
        with k.scope() as st2:
            moe_final(0, ya, st2)
    k.finish()

    return k


def _prep_inputs(inp):
    c = _host_consts()
    f = lambda a: np.ascontiguousarray(np.asarray(a, np.float32))
    shared = {
        "pos": c["pos"], "cst": c["cst"], "cstb": c["cstb"],
        "w_ada": f(inp["w_ada"][0]), "w_in": f(inp["w_in"][0]),
        "ln1_g": f(inp["ln1_g"]), "ln1_b": f(inp["ln1_b"]), "ln2_g": f(inp["ln2_g"]), "ln2_b": f(inp["ln2_b"]),
        "s5_a_re": f(inp["s5_a_re"][0]), "s5_a_im": f(inp["s5_a_im"][0]), "s5_log_dt": f(inp["s5_log_dt"][0]),
        "s5_b_re": f(inp["s5_b_re"][0]), "s5_b_im": f(inp["s5_b_im"][0]),
        "s5_c_re": f(inp["s5_c_re"][0]), "s5_c_im": f(inp["s5_c_im"][0]),
        "s5_w_glu": f(inp["s5_w_glu"][0]),
        "w_out": f(inp["w_out"][0]),
        "moe_wr": np.ascontiguousarray(np.concatenate([f(inp["moe_w_r1"][0]), f(inp["moe_w_r2"][0]).transpose(1, 0, 2).reshape(D, 16)], axis=1)),
        "moe_br": np.ascontiguousarray(np.concatenate([f(inp["moe_b_r1"][0]), f(inp["moe_b_r2"][0]).reshape(16)])[None, :]),
        "moe_w_gate": f(inp["moe_w_gate"][0]), "moe_w_up": f(inp["moe_w_up"][0]), "moe_w_down": f(inp["moe_w_down"][0]),
        "hy_f_w1": f(inp["hy_f_w1"][0]), "hy_f_w2": f(inp["hy_f_w2"][0]), "hy_f_w3": f(inp["hy_f_w3"][0]),
        "hy_f_b1": f(inp["hy_f_b1"][0]).reshape(64, 1), "hy_f_b2": f(inp["hy_f_b2"][0]).reshape(64, 1),
        "hy_freq": f(inp["hy_freq"][0]).reshape(64, 1), "hy_fbias": f(inp["hy_fbias"][0]).reshape(1, 1024),
        "zT1024": c["zT1024"], "zT256": c["zT256"], "dec1024": c["dec1024"], "dec256": c["dec256"],
        "dft1024": c["dft1024"], "dft256": c["dft256"],
    }
    maps = []
    for i in range(NCORE):
        m = dict(shared)
        m["xs"] = f(inp["x_sample"][i])
        m["xp"] = f(inp["x_prompt"][2 * i:2 * i + 2]).reshape(2 * LP, D)
        cc = np.stack([f(inp["c_ctx"]), f(inp["c"][i])], 0)
        vec = np.concatenate([cc.reshape(-1), f(inp["out_norm_g"][0]), f(inp["hy_conv_w"][0]).reshape(-1),
                              f(inp["hy_conv_b"][0]), f(inp["s5_d"][0]), f(inp["s5_b_glu"][0]), f(inp["b_ada"][0])])
        m["vecs"] = np.ascontiguousarray(vec.reshape(128, 128))
        m["h0"] = np.ascontiguousarray(np.stack([f(inp["state_s5_re"][i, 0]), f(inp["state_s5_im"][i, 0])], 0))
        maps.append(m)
    return maps


_CACHE = {}


def kernel(**inp):
    if "k" not in _CACHE:
        _CACHE["k"] = build()
    k = _CACHE["k"]
    maps = _prep_inputs(inp)
    res = run_bass_kernel_spmd(k.nc, maps, core_ids=list(range(NCORE)))
    r = res.results
    y_prompt = np.concatenate([r[i]["y_p"].reshape(2, LP, D) for i in range(NCORE)], 0).astype(np.float32)
    y_sample = np.stack([r[i]["y_s"] for i in range(NCORE)], 0).astype(np.float32)
    st_re = np.concatenate([r[i]["st_re"] for i in range(NCORE)], 0).reshape(2 * NCORE, 1, 2, G, P).astype(np.float32)
    st_im = np.concatenate([r[i]["st_im"] for i in range(NCORE)], 0).reshape(2 * NCORE, 1, 2, G, P).astype(np.float32)
    return (y_prompt, y_sample, st_re, st_im)
```
